# Optimizing a Trainium2 kernel written in Bass

```python
import jax, jax.numpy as jnp
from jax import lax
import numpy as np

D_MODEL = 1024
BATCH = 2
SEQ = 16384
DEPTH = 2

HEAD_DIM = 64
ATTN_GROUPS = ((128, 1), (512, 4), (2048, 16))
N_ATTN_GROUPS = 3
HEADS_PER_GROUP = 8
ATTN_WIDTH = N_ATTN_GROUPS * HEADS_PER_GROUP * HEAD_DIM
ATTN_OUT = HEADS_PER_GROUP * HEAD_DIM
BLK = 128
REL_BUCKETS = 32
REL_MAX_DIST = 2048
NEG_INF = -1e30
SSM_CH = 16
SSM_WIDTH = D_MODEL // 2
SSM_GROUPS = SSM_WIDTH // SSM_CH
SSM_STATE = 64
PROJ_WIDTH = 3 * ATTN_WIDTH + SSM_WIDTH + 2 * D_MODEL
D_FF = 11 * D_MODEL // 4
N_EXPERTS = 8
TOP_K = 2
D_FF_EXPERT = 7 * D_MODEL // 2
MOE_BLOCK = 512
N_DENSE = (DEPTH + 1) // 2
N_MOE = DEPTH // 2
EPS = 1e-6

kernel_name = "hybrid_dilated_attn_s5_moe_trunk"


def rmsnorm(x, g):
    x32 = x.astype(jnp.float32)
    y = x32 * lax.rsqrt(jnp.mean(x32 * x32, axis=-1, keepdims=True) + EPS)
    return (y * g.astype(jnp.float32)).astype(x.dtype)


def _t5_bucket(dist):
    max_exact = REL_BUCKETS // 2
    d = np.maximum(dist, 1).astype(np.float64)
    large = max_exact + (np.log(d / max_exact) / np.log(REL_MAX_DIST / max_exact)
                         * (REL_BUCKETS - max_exact)).astype(np.int32)
    large = np.minimum(large, REL_BUCKETS - 1)
    return np.where(dist < max_exact, dist, large).astype(np.int32)


def _dilated_group(q, k, v, table, window, dilation):
    B, S, H, Dh = q.shape
    r = dilation
    steps = window // dilation
    L = S // r
    nb = -(-L // BLK)
    Lp = nb * BLK

    def to_blocks(t):
        t = t.reshape(B, L, r, H, Dh).transpose(0, 2, 1, 3, 4)
        t = jnp.pad(t, ((0, 0), (0, 0), (0, Lp - L), (0, 0), (0, 0)))
        return t.reshape(B, r, nb, BLK, H, Dh)

    qb, kb, vb = to_blocks(q), to_blocks(k), to_blocks(v)

    def with_prev(t):
        prev = jnp.pad(t[:, :, :-1], ((0, 0), (0, 0), (1, 0), (0, 0), (0, 0), (0, 0)))
        return jnp.concatenate([prev, t], axis=3)

    kw, vw = with_prev(kb), with_prev(vb)

    qi = np.arange(BLK)[:, None]
    kj = np.arange(2 * BLK)[None, :]
    delta = BLK + qi - kj
    band = (delta >= 0) & (delta <= steps)
    first = (np.arange(nb) == 0)[:, None, None] & (kj < BLK)[None]
    valid = band[None] & ~first
    bucket = _t5_bucket(np.clip(delta, 0, steps) * r)
    bias = jnp.transpose(table[bucket], (2, 0, 1)).astype(jnp.float32)

    logits = jnp.einsum('bcnqhd,bcnkhd->bcnhqk', qb, kw).astype(jnp.float32) * (HEAD_DIM ** -0.5) + bias
    logits = jnp.where(valid[None, None, :, None], logits, NEG_INF)
    m = jnp.max(logits, axis=-1, keepdims=True)
    p = jnp.exp(logits - m)
    s = jnp.sum(p, axis=-1, keepdims=True)
    o = jnp.einsum('bcnhqk,bcnkhd->bcnqhd', (p / s).astype(v.dtype), vw)
    lse = (m + jnp.log(s))[..., 0]

    o = o.reshape(B, r, Lp, H, Dh)[:, :, :L].transpose(0, 2, 1, 3, 4).reshape(B, S, H, Dh)
    lse = lse.transpose(0, 1, 2, 4, 3).reshape(B, r, Lp, H)[:, :, :L].transpose(0, 2, 1, 3).reshape(B, S, H)
    return o, lse


def dilated_mixture(q, k, v, rel_bias):
    B, S, _ = q.shape
    shp = (B, S, N_ATTN_GROUPS, HEADS_PER_GROUP, HEAD_DIM)
    q, k, v = q.reshape(shp), k.reshape(shp), v.reshape(shp)
    outs, lses = [], []
    for g, (window, dilation) in enumerate(ATTN_GROUPS):
        table = rel_bias[:, g * HEADS_PER_GROUP:(g + 1) * HEADS_PER_GROUP]
        o, l = _dilated_group(q[:, :, g], k[:, :, g], v[:, :, g], table, window, dilation)
        outs.append(o)
        lses.append(l)
    alpha = jax.nn.softmax(jnp.stack(lses), axis=0)
    o = jnp.einsum('gbsh,gbshd->bshd', alpha, jnp.stack(outs).astype(jnp.float32))
    return o.reshape(B, S, ATTN_OUT).astype(q.dtype)


def s5_layer(u, lam_re, lam_im, log_dt, b_re, b_im, c_re, c_im, d_skip, w_glu, b_glu):
    Bsz, S, _ = u.shape
    f32 = jnp.float32
    ug = u.astype(f32).reshape(Bsz, S, SSM_GROUPS, SSM_CH)
    lam = lax.complex(lam_re.astype(f32), lam_im.astype(f32))
    dt = jnp.exp(log_dt.astype(f32))[:, None]
    lam_bar = jnp.exp(lam * dt)
    b = lax.complex(b_re.astype(f32), b_im.astype(f32))
    c = lax.complex(c_re.astype(f32), c_im.astype(f32))
    b_bar = ((lam_bar - 1.0) / lam)[..., None] * b
    bu = jnp.einsum('gpc,bsgc->bsgp', b_bar, ug.astype(jnp.complex64))
    a = jnp.broadcast_to(lam_bar, bu.shape)

    def combine(e1, e2):
        a1, x1 = e1
        a2, x2 = e2
        return a2 * a1, a2 * x1 + x2

    _, states = lax.associative_scan(combine, (a, bu), axis=1)
    y = jnp.einsum('gcp,bsgp->bsgc', c, states).real.reshape(Bsz, S, SSM_WIDTH)
    y = y + d_skip.astype(f32) * u.astype(f32)
    z = jax.nn.gelu(y).astype(u.dtype)
    return z * jax.nn.sigmoid(z @ w_glu + b_glu)


def hybrid_mixer(h, rel_bias, w_in, lam_re, lam_im, log_dt, b_re, b_im, c_re, c_im,
                 d_skip, w_glu, b_glu, w_attn_br, w_ssm_br, w_out):
    proj = h @ w_in
    cuts = np.cumsum([ATTN_WIDTH, ATTN_WIDTH, ATTN_WIDTH, SSM_WIDTH, D_MODEL])
    q, k, v, u, g_attn, g_ssm = jnp.split(proj, cuts, axis=-1)
    y_attn = dilated_mixture(q, k, v, rel_bias) @ w_attn_br
    y_ssm = s5_layer(u, lam_re, lam_im, log_dt, b_re, b_im, c_re, c_im, d_skip, w_glu, b_glu) @ w_ssm_br
    merged = jax.nn.sigmoid(g_attn) * y_attn + jax.nn.sigmoid(g_ssm) * y_ssm
    return merged @ w_out


def swiglu(h, w_gate, w_up, w_down):
    return (jax.nn.silu(h @ w_gate) * (h @ w_up)) @ w_down


def moe_swiglu(h, w_router, w_gate, w_up, w_down):
    N, D = h.shape
    logits = (h @ w_router).astype(jnp.float32)
    top_val, top_idx = lax.top_k(logits, TOP_K)
    gates = jax.nn.softmax(top_val, axis=-1)
    A = N * TOP_K
    flat_e = top_idx.reshape(-1)
    flat_tok = jnp.arange(A, dtype=jnp.int32) // TOP_K
    flat_g = gates.reshape(-1)
    order = jnp.argsort(flat_e)
    sorted_e = flat_e[order]
    counts = jnp.bincount(flat_e, length=N_EXPERTS)
    padded = ((counts + MOE_BLOCK - 1) // MOE_BLOCK) * MOE_BLOCK
    pad_end = jnp.cumsum(padded)
    pad_start = pad_end - padded
    start = jnp.cumsum(counts) - counts
    slot = pad_start[sorted_e] + (jnp.arange(A) - start[sorted_e])
    n_blocks = -(-A // MOE_BLOCK) + N_EXPERTS
    n_slots = n_blocks * MOE_BLOCK
    slot_tok = jnp.full((n_slots,), N, jnp.int32).at[slot].set(flat_tok[order])
    slot_gate = jnp.zeros((n_slots,), jnp.float32).at[slot].set(flat_g[order])
    block_e = jnp.minimum(jnp.searchsorted(pad_end, jnp.arange(n_blocks) * MOE_BLOCK, side='right'),
                          N_EXPERTS - 1)
    h_pad = jnp.concatenate([h, jnp.zeros((1, D), h.dtype)], axis=0)
    xs = h_pad[slot_tok].reshape(n_blocks, MOE_BLOCK, D)

    def block_fn(args):
        xb, e = args
        return (jax.nn.silu(xb @ w_gate[e]) * (xb @ w_up[e])) @ w_down[e]

    ys = lax.map(block_fn, (xs, block_e)).reshape(n_slots, D)
    ys = ys.astype(jnp.float32) * slot_gate[:, None]
    out = jnp.zeros((N + 1, D), jnp.float32).at[slot_tok].add(ys)[:N]
    return out.astype(h.dtype)


def setup_inputs(seed: int = 0) -> dict:
    key = jax.random.key(seed)
    ks = iter(jax.random.split(key, 32))
    nrm = lambda shape, scale: jax.random.normal(next(ks), shape, jnp.float32) * scale
    G, P, Hc = SSM_GROUPS, SSM_STATE, SSM_CH
    n_idx = jnp.arange(P, dtype=jnp.float32)
    return {
        "x": nrm((BATCH, SEQ, D_MODEL), 1.0),
        "rel_bias": nrm((REL_BUCKETS, N_ATTN_GROUPS * HEADS_PER_GROUP), 0.1),
        "norm1_g": 1.0 + nrm((DEPTH, D_MODEL), 0.02),
        "w_in": nrm((DEPTH, D_MODEL, PROJ_WIDTH), D_MODEL ** -0.5),
        "ssm_lam_re": -0.5 + nrm((DEPTH, G, P), 0.01),
        "ssm_lam_im": jnp.pi * n_idx + nrm((DEPTH, G, P), 0.01),
        "ssm_log_dt": jax.random.uniform(next(ks), (DEPTH, G), jnp.float32,
                                         float(np.log(1e-3)), float(np.log(1e-1))),
        "ssm_b_re": nrm((DEPTH, G, P, Hc), (2 * Hc) ** -0.5),
        "ssm_b_im": nrm((DEPTH, G, P, Hc), (2 * Hc) ** -0.5),
        "ssm_c_re": nrm((DEPTH, G, Hc, P), (2 * P) ** -0.5),
        "ssm_c_im": nrm((DEPTH, G, Hc, P), (2 * P) ** -0.5),
        "ssm_d": nrm((DEPTH, SSM_WIDTH), 1.0),
        "w_glu": nrm((DEPTH, SSM_WIDTH, SSM_WIDTH), SSM_WIDTH ** -0.5),
        "b_glu": nrm((DEPTH, SSM_WIDTH), 0.01),
        "w_attn_br": nrm((DEPTH, ATTN_OUT, D_MODEL), ATTN_OUT ** -0.5),
        "w_ssm_br": nrm((DEPTH, SSM_WIDTH, D_MODEL), SSM_WIDTH ** -0.5),
        "w_out": nrm((DEPTH, D_MODEL, D_MODEL), D_MODEL ** -0.5),
        "norm2_g": 1.0 + nrm((DEPTH, D_MODEL), 0.02),
        "ffn_w_gate": nrm((N_DENSE, D_MODEL, D_FF), D_MODEL ** -0.5),
        "ffn_w_up": nrm((N_DENSE, D_MODEL, D_FF), D_MODEL ** -0.5),
        "ffn_w_down": nrm((N_DENSE, D_FF, D_MODEL), D_FF ** -0.5),
        "moe_router": nrm((N_MOE, D_MODEL, N_EXPERTS), D_MODEL ** -0.5),
        "moe_w_gate": nrm((N_MOE, N_EXPERTS, D_MODEL, D_FF_EXPERT), D_MODEL ** -0.5),
        "moe_w_up": nrm((N_MOE, N_EXPERTS, D_MODEL, D_FF_EXPERT), D_MODEL ** -0.5),
        "moe_w_down": nrm((N_MOE, N_EXPERTS, D_FF_EXPERT, D_MODEL), D_FF_EXPERT ** -0.5),
        "final_norm_g": 1.0 + nrm((D_MODEL,), 0.02),
    }


def reference(x, rel_bias, norm1_g, w_in, ssm_lam_re, ssm_lam_im, ssm_log_dt, ssm_b_re, ssm_b_im,
              ssm_c_re, ssm_c_im, ssm_d, w_glu, b_glu, w_attn_br, w_ssm_br, w_out, norm2_g,
              ffn_w_gate, ffn_w_up, ffn_w_down, moe_router, moe_w_gate, moe_w_up, moe_w_down,
              final_norm_g):
    B, S, D = x.shape
    for l in range(DEPTH):
        h = rmsnorm(x, norm1_g[l])
        x = x + hybrid_mixer(h, rel_bias, w_in[l], ssm_lam_re[l], ssm_lam_im[l], ssm_log_dt[l],
                             ssm_b_re[l], ssm_b_im[l], ssm_c_re[l], ssm_c_im[l], ssm_d[l],
                             w_glu[l], b_glu[l], w_attn_br[l], w_ssm_br[l], w_out[l])
        h = rmsnorm(x, norm2_g[l])
        if l % 2 == 0:
            i = l // 2
            x = x + swiglu(h, ffn_w_gate[i], ffn_w_up[i], ffn_w_down[i])
        else:
            i = l // 2
            x = x + moe_swiglu(h.reshape(B * S, D), moe_router[i], moe_w_gate[i], moe_w_up[i],
                               moe_w_down[i]).reshape(B, S, D)
    return rmsnorm(x, final_norm_g)
```

```python
import numpy as np
import ml_dtypes
from concourse.bass_utils import run_bass_kernel_spmd
import concourse.bass as bass
import concourse.mybir as mybir
from contextlib import ExitStack

F32 = mybir.dt.float32
BF16 = mybir.dt.bfloat16
I32 = mybir.dt.int32
ALU = mybir.AluOpType
AF = mybir.ActivationFunctionType
AX = mybir.AxisListType

ENGS = ("pe", "act", "dve", "pool", "sp")
NDSEM = 8


class Buf:
    __slots__ = ("name", "last_w", "readers")

    def __init__(self, name=""):
        self.name = name
        self.last_w = None
        self.readers = []


class Op:
    __slots__ = ("eng", "fn", "deps", "is_dma", "signal", "sig_val", "dsem", "dval", "dprev", "idx")

    def __init__(self, eng, fn, deps, is_dma):
        self.eng = eng
        self.fn = fn
        self.deps = deps
        self.is_dma = is_dma
        self.signal = False
        self.sig_val = 0
        self.dsem = None
        self.dval = 0
        self.dprev = 0
        self.idx = 0


class Prog:
    def __init__(self, nc, arena_bytes=0):
        self.nc = nc
        self.q = {e: [] for e in ENGS}
        self.es = ExitStack()
        self.nops = 0
        self.arena = None
        self.phase_dmas = []
        if arena_bytes:
            self.arena = self.es.enter_context(nc.sbuf_tensor("arena_all", [128, arena_bytes // 2], BF16))
            self.parena = self.es.enter_context(nc.psum_tensor("psum_all", [128, 4096], F32))
            self.arena_bytes = arena_bytes
            self.off = 0
            self.poff = 0

    @staticmethod
    def _shape_view(flat, shape):
        if len(shape) == 2:
            return flat
        names = " ".join("d%d" % i for i in range(1, len(shape)))
        kw = {"d%d" % i: int(shape[i]) for i in range(1, len(shape) - 1)}
        return flat.rearrange("p (%s) -> p %s" % (names, names), **kw)

    def sb(self, name, shape, dt):
        if self.arena is None:
            return self.es.enter_context(self.nc.sbuf_tensor(name, list(shape), dt))
        assert shape[0] == 128
        esz = 2 if dt == BF16 else 4
        n = int(np.prod(shape[1:]))
        nbytes = (n * esz + 63) // 64 * 64
        assert self.off + nbytes <= self.arena_bytes, "SBUF arena overflow %s %d" % (name, self.off + nbytes)
        flat = self.arena[:, self.off // 2:self.off // 2 + n * esz // 2]
        self.off += nbytes
        if dt != BF16:
            flat = flat.bitcast(dt)
        return self._shape_view(flat, shape)

    def ps(self, name, shape, dt=F32):
        if self.arena is None:
            return self.es.enter_context(self.nc.psum_tensor(name, list(shape), dt))
        n = int(np.prod(shape[1:]))
        nb = (n + 511) // 512
        assert self.poff + nb * 512 <= 4096, "PSUM arena overflow"
        flat = self.parena[:, self.poff:self.poff + n]
        self.poff += nb * 512
        return self._shape_view(flat, shape)

    def phase_end(self):
        deps = list(self.phase_dmas)
        for e in ENGS:
            for o in reversed(self.q[e]):
                if not o.is_dma and o.fn is not None:
                    deps.append(o)
                    break
        for e in ENGS:
            self.op(e, None, deps=deps)
        self.phase_dmas = []
        self.off = 0
        self.poff = 0

    def op(self, eng, fn, reads=(), writes=(), deps=(), dma=False):
        d = [x for x in deps if x is not None]
        for b in reads:
            if b.last_w is not None:
                d.append(b.last_w)
        for b in writes:
            if b.last_w is not None:
                d.append(b.last_w)
            d.extend(b.readers)
        o = Op(eng, fn, d, dma)
        if dma:
            self.phase_dmas.append(o)
        o.idx = self.nops
        self.nops += 1
        self.q[eng].append(o)
        for b in reads:
            b.readers.append(o)
        for b in writes:
            b.last_w = o
            b.readers = []
        return o

    def pe(self, fn, reads=(), writes=(), deps=()):
        return self.op("pe", fn, reads, writes, deps)

    def act(self, fn, reads=(), writes=(), deps=()):
        return self.op("act", fn, reads, writes, deps)

    def dve(self, fn, reads=(), writes=(), deps=()):
        return self.op("dve", fn, reads, writes, deps)

    def pool(self, fn, reads=(), writes=(), deps=()):
        return self.op("pool", fn, reads, writes, deps)

    def dma(self, eng, out, in_, reads=(), writes=(), deps=(), **kw):
        return self.op(eng, lambda e: e.dma_start(out=out, in_=in_, **kw), reads, writes, deps, dma=True)

    def fence(self, eng, deps):
        return self.op(eng, None, (), (), deps)

    def emit(self):
        nc = self.nc
        for e in ENGS:
            for o in self.q[e]:
                for d in o.deps:
                    if d.is_dma or d.eng != o.eng or o.eng != "pe":
                        d.signal = True
        for e in ENGS:
            cnt = 0
            dcnt = 0
            duse = [0] * NDSEM
            for o in self.q[e]:
                if o.is_dma:
                    k = dcnt % NDSEM
                    dcnt += 1
                    o.dsem = k
                    o.dprev = duse[k]
                    duse[k] += 16
                    o.dval = duse[k]
                elif o.signal:
                    cnt += 1
                    o.sig_val = cnt
        sems = {e: self.es.enter_context(nc.semaphore("s_" + e)) for e in ENGS}
        dsems = {e: [self.es.enter_context(nc.semaphore("d_%s%d" % (e, k))) for k in range(NDSEM)]
                 for e in ("sp", "act", "pool")}
        block = self.es.enter_context(nc.Block())

        def run(ename, eng):
            waited = {}

            def wait(sem, key, val):
                if waited.get(key, 0) >= val:
                    return
                waited[key] = val
                eng.wait_ge(sem, val)

            for o in self.q[ename]:
                for d in o.deps:
                    if d.is_dma:
                        wait(dsems[d.eng][d.dsem], ("d", d.eng, d.dsem), d.dval)
                    elif d.eng != ename or ename != "pe":
                        wait(sems[d.eng], ("e", d.eng), d.sig_val)
                if o.is_dma:
                    if o.dprev:
                        wait(dsems[ename][o.dsem], ("d", ename, o.dsem), o.dprev)
                    ins = o.fn(eng)
                    ins.then_inc(dsems[ename][o.dsem], 16)
                elif o.fn is not None:
                    ins = o.fn(eng)
                    if o.signal:
                        ins.then_inc(sems[ename], 1)

        @block.tensor
        def _(eng):
            run("pe", eng)

        @block.scalar
        def _(eng):
            run("act", eng)

        @block.vector
        def _(eng):
            run("dve", eng)

        @block.gpsimd
        def _(eng):
            run("pool", eng)

        @block.sync
        def _(eng):
            run("sp", eng)

    def close(self):
        self.es.close()


TOK = 4096
D = 1024
EPS = 1e-6
GROUPS = ((128, 1), (512, 4), (2048, 16))
PI = float(np.pi)


def tok_slices(r, n):
    L = TOK // r
    n = min(n, L)
    out = []
    for c in range(r):
        for m0 in range(0, L, n):
            out.append((c * L + m0, slice(c + r * m0, c + r * (m0 + n - 1) + 1, r)))
    return out


_OVR = None
_PROG = None


def _dram(nc, name, shape, dt, kind):
    if _OVR is not None and name in _OVR:
        return _OVR[name]
    assert _OVR is None, "fused mode: missing DRAM binding for " + name
    return nc.dram_tensor(name, list(shape), dt, kind=kind).ap()


def _prog(nc):
    return _PROG if _PROG is not None else Prog(nc)


def _finish(P):
    if _PROG is not None:
        P.phase_end()
    else:
        P.emit()
        P.close()


def emit_lb(P, lamre, lamim, logdt, tmp, LR, LI, rb, wb, rho=None, cs=None):
    t0, t1, t2, t3 = tmp[0], tmp[1], tmp[2], tmp[3]
    dv = lambda f: P.dve(f, reads=rb, writes=wb)
    ac = lambda f: P.act(f, reads=rb, writes=wb)
    ac(lambda e: e.activation(out=t0, in_=logdt, func=AF.Exp))
    dv(lambda e: e.tensor_tensor(out=t1, in0=lamre, in1=t0, op=ALU.mult))
    dv(lambda e: e.tensor_tensor(out=t2, in0=lamim, in1=t0, op=ALU.mult))
    ac(lambda e: e.activation(out=t1, in_=t1, func=AF.Exp))
    dv(lambda e: e.tensor_scalar(out=t0, in0=t2, scalar1=1.0 / (2 * PI), scalar2=None, op0=ALU.mult))
    dv(lambda e: e.tensor_copy(out=t3.bitcast(I32), in_=t0))
    dv(lambda e: e.tensor_copy(out=t0, in_=t3.bitcast(I32)))
    dv(lambda e: e.scalar_tensor_tensor(out=t2, in0=t0, scalar=-2 * PI, in1=t2, op0=ALU.mult, op1=ALU.add))
    ac(lambda e: e.activation(out=t0, in_=t2, func=AF.Sin, scale=0.5))
    ac(lambda e: e.activation(out=t3, in_=t2, func=AF.Sin, scale=0.25))
    dv(lambda e: e.tensor_tensor(out=t2, in0=t0, in1=t0, op=ALU.mult))
    dv(lambda e: e.tensor_scalar(out=t2, in0=t2, scalar1=-2.0, scalar2=1.0, op0=ALU.mult, op1=ALU.add))
    dv(lambda e: e.tensor_tensor(out=t3, in0=t3, in1=t3, op=ALU.mult))
    dv(lambda e: e.tensor_scalar(out=t3, in0=t3, scalar1=-2.0, scalar2=1.0, op0=ALU.mult, op1=ALU.add))
    dv(lambda e: e.scalar_tensor_tensor(out=t3, in0=t0, scalar=2.0, in1=t3, op0=ALU.mult, op1=ALU.mult))
    dv(lambda e: e.tensor_tensor(out=t0, in0=t2, in1=t2, op=ALU.mult))
    dv(lambda e: e.tensor_tensor(out=LR, in0=t3, in1=t3, op=ALU.mult))
    dv(lambda e: e.tensor_tensor(out=LR, in0=t0, in1=LR, op=ALU.add))
    ac(lambda e: e.activation(out=t0, in_=LR, func=AF.Sqrt))
    dv(lambda e: e.reciprocal(out=t0, in_=t0))
    dv(lambda e: e.tensor_tensor(out=LI, in0=t0, in1=t0, op=ALU.mult))
    dv(lambda e: e.tensor_tensor(out=LI, in0=LI, in1=LR, op=ALU.mult))
    dv(lambda e: e.tensor_scalar(out=LI, in0=LI, scalar1=-0.5, scalar2=1.5, op0=ALU.mult, op1=ALU.add))
    dv(lambda e: e.tensor_tensor(out=t0, in0=t0, in1=LI, op=ALU.mult))
    dv(lambda e: e.tensor_tensor(out=t2, in0=t2, in1=t0, op=ALU.mult))
    dv(lambda e: e.tensor_tensor(out=t3, in0=t3, in1=t0, op=ALU.mult))
    if rho is not None:
        dv(lambda e: e.tensor_copy(out=rho, in_=t1))
        dv(lambda e: e.tensor_copy(out=cs[0], in_=t2))
        dv(lambda e: e.tensor_copy(out=cs[1], in_=t3))
    dv(lambda e: e.tensor_tensor(out=LR, in0=t1, in1=t2, op=ALU.mult))
    dv(lambda e: e.tensor_tensor(out=LI, in0=t1, in1=t3, op=ALU.mult))


def build_A(nc, mode="full", prev=False):
    x = _dram(nc, "x", [TOK, D], F32, "ExternalInput")
    gbc = _dram(nc, "gbc", [128, D], F32, "ExternalInput")
    w_in = _dram(nc, "w_in", [D, 7168], F32, "ExternalInput")
    ident = _dram(nc, "ident", [128, 128], F32, "ExternalInput")
    ssmB = _dram(nc, "ssmB", [128, 5, 4, 64], F32, "ExternalInput")
    ssmP = _dram(nc, "ssmP", [128, 3, 16], F32, "ExternalInput")
    ssmC = _dram(nc, "ssmC", [128, 2, 16, 16], F32, "ExternalInput")
    dsk = _dram(nc, "dsk", [128, 4], F32, "ExternalInput")
    masks = _dram(nc, "masks", [128, 8], F32, "ExternalInput")
    qT = _dram(nc, "qT", [3, 512, TOK], BF16, "ExternalOutput")
    kT = _dram(nc, "kT", [3, 512, TOK], BF16, "ExternalOutput")
    vv = _dram(nc, "v", [3, TOK, 512], BF16, "ExternalOutput")
    sgT = _dram(nc, "sgT", [2048, TOK], F32, "ExternalOutput")
    ylocT = _dram(nc, "ylocT", [512, TOK], F32, "ExternalOutput")
    Fo = _dram(nc, "F", [128, 16, 2], F32, "ExternalOutput")
    Fin = _dram(nc, "Fin", [128, 16, 2], F32, "ExternalInput") if prev else None
    DBG = False
    if DBG:
        dbg1 = _dram(nc, "dbg_LK", [128, 3, 1, 16], F32, "ExternalOutput")
        dbg2 = _dram(nc, "dbg_tB", [128, 10, 4, 64], F32, "ExternalOutput")
        dbg3 = _dram(nc, "dbg_tP", [128, 8, 16], F32, "ExternalOutput")
        dbg4 = _dram(nc, "dbg_sP", [128, 3, 16], F32, "ExternalOutput")

    P = _prog(nc)
    arena = P.sb("arena", [128, 32768], BF16)
    hT = arena[:].rearrange("p (k t) -> p k t", k=8)
    scan = arena[:].bitcast(F32).rearrange("p (a h t) -> p a h t", a=2, h=2)
    uT = P.sb("uT", [128, 4, TOK], BF16)
    warena = P.sb("warena", [128, 8192], BF16)
    wbuf = [warena[:, i * 4096:(i + 1) * 4096].rearrange("p (k c) -> p k c", k=8) for i in range(2)]
    Xb = warena[:].rearrange("p (h t) -> p h t", h=2)
    stgbf_t = P.sb("stgbf", [128, 2, TOK], BF16)
    stg_bf = [stgbf_t[:, i, :] for i in range(2)]
    T1 = stgbf_t[:].rearrange("p a t -> p (a t)").bitcast(F32)
    stgf_t = P.sb("stgf", [128, 2, TOK], F32)
    stg_f = [stgf_t[:, i, :] for i in range(2)]
    xt = [stgf_t[:, 0, i * 1024:(i + 1) * 1024] for i in range(2)]
    xs = [stgf_t[:, 0, (2 + i) * 1024:(3 + i) * 1024] for i in range(2)]
    junk = stgf_t[:, 1, 0:1024]
    gb = stgf_t[:, 1, 1024:2048]
    idf = P.sb("idf", [128, 128], F32)
    small = P.sb("small", [128, 8], F32)
    sBt = P.sb("sBt", [128, 5, 4, 64], F32)
    tB = P.sb("tB", [128, 10, 4, 64], F32)
    sPt = P.sb("sPt", [128, 3, 16], F32)
    tP = P.sb("tP", [128, 8, 16], F32)
    LK = P.sb("LK", [128, 3, 1, 16], F32)
    WK = P.sb("WK", [128, 3, 12, 16], F32)
    RHO = P.sb("RHO", [128, 16], F32)
    Fpv = P.sb("Fpv", [128, 16, 2], F32)
    INI = P.sb("INI", [128, 2, 16], F32)
    sCt = P.sb("sCt", [128, 2, 16, 16], F32)
    dskt = P.sb("dskt", [128, 4], F32)
    mskt = P.sb("mskt", [128, 8], F32)
    Bblk = P.sb("Bblk", [128, 16, 2, 128], BF16)
    Cblk = P.sb("Cblk", [128, 16, 2, 128], BF16)
    Fsb = P.sb("Fsb", [128, 16, 2], F32)
    vstg = [P.sb("vstg%d" % i, [128, 4, 512], BF16) for i in range(2)]
    tp = [P.ps("tp%d" % i, [128, 8, 128]) for i in range(2)]
    pb = [P.ps("pb%d" % i, [128, 512]) for i in range(4)]

    Bxt = [Buf(), Buf()]; Bxs = [Buf(), Buf()]; Bsm = [Buf(), Buf()]; Btp = [Buf(), Buf()]
    Bjunk = Buf(); Bgb = Buf(); Bid = Buf(); BhT = [Buf(), Buf()]; Bpb = [Buf() for _ in range(4)]
    Bw = [Buf(), Buf()]; Bsbf = [Buf(), Buf()]; Bsf = [Buf(), Buf()]; BuT = Buf(); Bvs = [Buf(), Buf()]
    Bpar = Buf(); BBblk = Buf(); BCblk = Buf(); BF_ = Buf(); BXb = Buf()
    BA = [[Buf(), Buf()], [Buf(), Buf()]]
    outs = []

    P.dma("act", idf[:], ident, writes=[Bid])
    P.dma("act", gb, gbc, writes=[Bgb])
    P.dma("act", sBt[:], ssmB, writes=[Bpar])
    P.dma("act", sPt[:], ssmP, writes=[Bpar])
    P.dma("act", sCt[:], ssmC, writes=[Bpar])
    P.dma("act", dskt[:], dsk, writes=[Bpar])
    P.dma("act", mskt[:], masks, writes=[Bpar])

    def ph1(tt):
        b = tt % 2
        ss = small[:, b:b + 1]
        rs = small[:, 2 + b:3 + b]
        P.dma("sp", xt[b], x[tt * 128:(tt + 1) * 128, :], writes=[Bxt[b]])
        P.act(lambda e: e.activation(out=junk, in_=xt[b], func=AF.Square), reads=[Bxt[b]], writes=[Bjunk])
        P.dve(lambda e: e.reduce_sum(out=ss, in_=junk, axis=AX.X), reads=[Bjunk], writes=[Bsm[b]])
        P.act(lambda e: e.activation(out=rs, in_=ss, func=AF.Sqrt, bias=EPS, scale=1.0 / D), writes=[Bsm[b]])
        P.dve(lambda e: e.reciprocal(out=rs, in_=rs), writes=[Bsm[b]])
        P.dve(lambda e: e.scalar_tensor_tensor(out=xs[b], in0=xt[b], scalar=rs, in1=gb, op0=ALU.mult, op1=ALU.mult),
              reads=[Bxt[b], Bsm[b], Bgb], writes=[Bxs[b]])

        def tr(e):
            for kc in range(8):
                ins = e.transpose(out=tp[b][:, kc, :], in_=xs[b][:, kc * 128:(kc + 1) * 128], identity=idf[:])
            return ins
        P.pe(tr, reads=[Bxs[b], Bid], writes=[Btp[b]])
        P.act(lambda e: e.copy(out=hT[:, 0:4, tt * 128:(tt + 1) * 128], in_=tp[b][:, 0:4, :]),
              reads=[Btp[b]], writes=[BhT[0]])
        P.dve(lambda e: e.tensor_copy(out=hT[:, 4:8, tt * 128:(tt + 1) * 128], in_=tp[b][:, 4:8, :]),
              reads=[Btp[b]], writes=[BhT[1]])
    for tt in range(TOK // 128):
        ph1(tt)

    cnt = {"pb": 0, "ev": 0, "sb": 0, "sf": 0, "vs": 0}

    def mm_group(lhs_fn, rhs_fn, n):
        i = cnt["pb"] % 4
        cnt["pb"] += 1

        def f(e):
            for kc in range(8):
                ins = e.matmul(pb[i][:, 0:n], lhsT=lhs_fn(kc), rhs=rhs_fn(kc), start=(kc == 0), stop=(kc == 7))
            return ins
        return i, f

    def load_w(cg):
        P.dma("pool", wbuf[cg % 2], w_in[:, cg * 512:(cg + 1) * 512].rearrange("(k p) c -> p k c", p=128),
              writes=[Bw[cg % 2]])

    cgs = {"full": list(range(14)), "kvu": [3, 4, 5, 6, 7, 8, 9], "u": [9]}[mode]
    load_w(cgs[0])
    for ci_, cg in enumerate(cgs):
        if ci_ + 1 < len(cgs):
            load_w(cgs[ci_ + 1])
        wb = wbuf[cg % 2]
        Bwc = Bw[cg % 2]
        if cg in (6, 7, 8):
            g = cg - 6
            r = GROUPS[g][1]
            for ti, (pos0, tsl) in enumerate(tok_slices(r, 128)):
                s = (cnt["vs"] // 4) % 2
                a = cnt["vs"] % 4
                cnt["vs"] += 1
                i, f = mm_group(lambda kc, tsl=tsl: hT[:, kc, tsl], lambda kc, wb=wb: wb[:, kc, :], 512)
                P.pe(f, reads=[BhT[0], BhT[1], Bwc], writes=[Bpb[i]])
                if ti % 2 == 0:
                    P.act(lambda e, i=i, s=s, a=a: e.copy(out=vstg[s][:, a, :], in_=pb[i][:]),
                          reads=[Bpb[i]], writes=[Bvs[s]])
                else:
                    P.dve(lambda e, i=i, s=s, a=a: e.tensor_copy(out=vstg[s][:, a, :], in_=pb[i][:]),
                          reads=[Bpb[i]], writes=[Bvs[s]])
                if a == 3:
                    outs.append(P.dma("sp", vv[g, pos0 - 384:pos0 + 128, :].rearrange("(a p) c -> p a c", p=128),
                                      vstg[s][:], reads=[Bvs[s]]))
            continue
        if cg < 6:
            r = GROUPS[cg % 3][1]
        else:
            r = 1
        for cc in range(4):
            if cg < 6:
                s = cnt["sb"] % 2
                cnt["sb"] += 1
                dst, Bdst = stg_bf[s], Bsbf[s]
            elif cg == 9:
                dst, Bdst = uT[:, cc, :], BuT
            else:
                s = cnt["sf"] % 2
                cnt["sf"] += 1
                dst, Bdst = stg_f[s], Bsf[s]
            for (pos0, tsl) in tok_slices(r, 512):
                n = min(512, TOK // r)
                i, f = mm_group(lambda kc, wb=wb, cc=cc: wb[:, kc, cc * 128:(cc + 1) * 128],
                                lambda kc, tsl=tsl: hT[:, kc, tsl], n)
                P.pe(f, reads=[BhT[0], BhT[1], Bwc], writes=[Bpb[i]])
                o = dst[:, pos0:pos0 + n]
                if cg >= 10:
                    P.act(lambda e, i=i, o=o, n=n: e.activation(out=o, in_=pb[i][:, 0:n], func=AF.Sigmoid),
                          reads=[Bpb[i]], writes=[Bdst])
                else:
                    P.dve(lambda e, i=i, o=o, n=n: e.tensor_copy(out=o, in_=pb[i][:, 0:n]),
                          reads=[Bpb[i]], writes=[Bdst])
            if cg < 3:
                outs.append(P.dma("sp", qT[cg, cc * 128:(cc + 1) * 128, :], dst, reads=[Bdst]))
            elif cg < 6:
                outs.append(P.dma("sp", kT[cg - 3, cc * 128:(cc + 1) * 128, :], dst, reads=[Bdst]))
            elif cg >= 10:
                row = (cg - 10) * 512 + cc * 128
                outs.append(P.dma("sp", sgT[row:row + 128, :], dst, reads=[Bdst]))

    rb, wbp = [Bpar], [Bpar]
    T = lambda i: tB[:, i]
    emit_lb(P, sBt[:, 0], sBt[:, 1], sBt[:, 2], [T(0), T(1), T(2), T(3)], T(4), T(5), rb, wbp)
    LRb, LIb, lre, lim, bre, bim = T(4), T(5), sBt[:, 0], sBt[:, 1], sBt[:, 3], sBt[:, 4]
    dv = lambda f: P.dve(f, reads=rb, writes=wbp)
    dv(lambda e: e.tensor_scalar(out=T(0), in0=LRb, scalar1=-1.0, scalar2=None, op0=ALU.add))
    dv(lambda e: e.tensor_tensor(out=T(1), in0=lre, in1=lre, op=ALU.mult))
    dv(lambda e: e.tensor_tensor(out=T(2), in0=lim, in1=lim, op=ALU.mult))
    dv(lambda e: e.tensor_tensor(out=T(1), in0=T(1), in1=T(2), op=ALU.add))
    dv(lambda e: e.reciprocal(out=T(1), in_=T(1)))
    dv(lambda e: e.tensor_tensor(out=T(2), in0=T(0), in1=lre, op=ALU.mult))
    dv(lambda e: e.tensor_tensor(out=T(3), in0=LIb, in1=lim, op=ALU.mult))
    dv(lambda e: e.tensor_tensor(out=T(2), in0=T(2), in1=T(3), op=ALU.add))
    dv(lambda e: e.tensor_tensor(out=T(3), in0=LIb, in1=lre, op=ALU.mult))
    dv(lambda e: e.tensor_tensor(out=T(6), in0=T(0), in1=lim, op=ALU.mult))
    dv(lambda e: e.tensor_tensor(out=T(3), in0=T(3), in1=T(6), op=ALU.subtract))
    dv(lambda e: e.tensor_tensor(out=T(2), in0=T(2), in1=T(1), op=ALU.mult))
    dv(lambda e: e.tensor_tensor(out=T(3), in0=T(3), in1=T(1), op=ALU.mult))
    dv(lambda e: e.tensor_tensor(out=T(6), in0=T(2), in1=bre, op=ALU.mult))
    dv(lambda e: e.tensor_tensor(out=T(7), in0=T(3), in1=bim, op=ALU.mult))
    dv(lambda e: e.tensor_tensor(out=T(8), in0=T(6), in1=T(7), op=ALU.subtract))
    dv(lambda e: e.tensor_tensor(out=T(6), in0=T(2), in1=bim, op=ALU.mult))
    dv(lambda e: e.tensor_tensor(out=T(7), in0=T(3), in1=bre, op=ALU.mult))
    dv(lambda e: e.tensor_tensor(out=T(9), in0=T(6), in1=T(7), op=ALU.add))
    for Pp in range(16):
        gq, q = Pp // 4, Pp % 4
        for h in range(2):
            src = T(8 + h)[:, gq, :]
            P.dve(lambda e, Pp=Pp, h=h, q=q, src=src: e.tensor_scalar(
                out=Bblk[:, Pp, h, 0:64], in0=src, scalar1=mskt[:, q:q + 1], scalar2=None, op0=ALU.mult),
                reads=rb, writes=[BBblk])
            P.dve(lambda e, Pp=Pp, h=h, q=q, src=src: e.tensor_scalar(
                out=Bblk[:, Pp, h, 64:128], in0=src, scalar1=mskt[:, 4 + q:5 + q], scalar2=None, op0=ALU.mult),
                reads=rb, writes=[BBblk])
    TP = lambda i: tP[:, i]
    emit_lb(P, sPt[:, 0], sPt[:, 1], sPt[:, 2], [TP(0), TP(1), TP(2), TP(3)], LK[:, 0, 0], LK[:, 1, 0], rb, wbp,
            rho=RHO[:], cs=(WK[:, 0, 0], WK[:, 2, 0]))
    dv(lambda e: e.tensor_scalar(out=WK[:, 1, 0], in0=WK[:, 2, 0], scalar1=-1.0, scalar2=None, op0=ALU.mult))
    for k in range(11):
        dv(lambda e, k=k: e.tensor_tensor(out=TP(0), in0=WK[:, 0, k], in1=WK[:, 0, k], op=ALU.mult))
        dv(lambda e, k=k: e.tensor_tensor(out=TP(1), in0=WK[:, 1, k], in1=WK[:, 1, k], op=ALU.mult))
        dv(lambda e, k=k: e.tensor_tensor(out=WK[:, 0, k + 1], in0=TP(0), in1=TP(1), op=ALU.subtract))
        dv(lambda e, k=k: e.scalar_tensor_tensor(out=WK[:, 1, k + 1], in0=WK[:, 0, k], scalar=2.0, in1=WK[:, 1, k],
                                                  op0=ALU.mult, op1=ALU.mult))
    dv(lambda e: e.tensor_scalar(out=WK[:, 2], in0=WK[:, 1], scalar1=-1.0, scalar2=None, op0=ALU.mult))
    if prev:
        P.dma("act", Fpv[:], Fin, writes=[Bpar])
        dv(lambda e: e.tensor_tensor(out=TP(0), in0=WK[:, 0, 0], in1=Fpv[:, :, 0], op=ALU.mult))
        dv(lambda e: e.tensor_tensor(out=TP(1), in0=WK[:, 2, 0], in1=Fpv[:, :, 1], op=ALU.mult))
        dv(lambda e: e.tensor_tensor(out=INI[:, 0], in0=TP(0), in1=TP(1), op=ALU.subtract))
        dv(lambda e: e.tensor_tensor(out=TP(0), in0=WK[:, 2, 0], in1=Fpv[:, :, 0], op=ALU.mult))
        dv(lambda e: e.tensor_tensor(out=TP(1), in0=WK[:, 0, 0], in1=Fpv[:, :, 1], op=ALU.mult))
        dv(lambda e: e.tensor_tensor(out=INI[:, 1], in0=TP(0), in1=TP(1), op=ALU.add))
    else:
        dv(lambda e: e.memset(INI[:], 0.0))
    P.dve(lambda e: e.memset(Cblk[:], 0.0), writes=[BCblk])
    for Pp in range(16):
        q = Pp % 4
        for g2 in range(2):
            c0 = 32 * q + 16 * g2
            ps_ = slice(64 * g2, 64 * g2 + 64)
            P.dve(lambda e, Pp=Pp, c0=c0, ps_=ps_: e.tensor_copy(out=Cblk[ps_, Pp, 0, c0:c0 + 16], in_=sCt[ps_, 0, Pp, :]),
                  reads=rb, writes=[BCblk])
            P.dve(lambda e, Pp=Pp, c0=c0, ps_=ps_: e.tensor_scalar(
                out=Cblk[ps_, Pp, 1, c0:c0 + 16], in0=sCt[ps_, 1, Pp, :], scalar1=-1.0, scalar2=None, op0=ALU.mult),
                reads=rb, writes=[BCblk])

    for Pp in range(16):
        gq, q = Pp // 4, Pp % 4
        for tg in range(8):
            for h in range(2):
                i = cnt["pb"] % 4
                cnt["pb"] += 1
                P.pe(lambda e, i=i, Pp=Pp, h=h, tg=tg, gq=gq: e.matmul(
                    pb[i][:], lhsT=Bblk[:, Pp, h, :], rhs=uT[:, gq, tg * 512:(tg + 1) * 512], start=True, stop=True),
                    reads=[BBblk, BuT], writes=[Bpb[i]])
                P.act(lambda e, i=i, h=h, tg=tg: e.copy(out=scan[:, 0, h, tg * 512:(tg + 1) * 512], in_=pb[i][:]),
                      reads=[Bpb[i]], writes=[BA[0][h]])
        Sr, Si, Wr, Wi = scan[:, 0, 0], scan[:, 0, 1], scan[:, 1, 0], scan[:, 1, 1]
        T2 = stg_f[1]
        BSr, BSi, BW, BT1, BT2 = BA[0][0], BA[0][1], BA[1][0], [Bsbf[0], Bsbf[1]], Bsf[1]
        wd_ = lambda f: P.dve(f, reads=[Bpar], writes=[BW])
        wd_(lambda e: e.memset(Wr[:, 0:1], 1.0))
        wd_(lambda e: e.memset(Wi[:, 0:1], 0.0))
        for s_ in range(12):
            k = 1 << s_
            wr, wi, nwi = WK[:, 0, s_, Pp:Pp + 1], WK[:, 1, s_, Pp:Pp + 1], WK[:, 2, s_, Pp:Pp + 1]
            wd_(lambda e, k=k, wr=wr: e.tensor_scalar(out=Wr[:, k:2 * k], in0=Wr[:, 0:k], scalar1=wr, scalar2=None,
                                                      op0=ALU.mult))
            wd_(lambda e, k=k, nwi=nwi: e.scalar_tensor_tensor(out=Wr[:, k:2 * k], in0=Wi[:, 0:k], scalar=nwi,
                                                               in1=Wr[:, k:2 * k], op0=ALU.mult, op1=ALU.add))
            wd_(lambda e, k=k, wi=wi: e.tensor_scalar(out=Wi[:, k:2 * k], in0=Wr[:, 0:k], scalar1=wi, scalar2=None,
                                                      op0=ALU.mult))
            wd_(lambda e, k=k, wr=wr: e.scalar_tensor_tensor(out=Wi[:, k:2 * k], in0=Wi[:, 0:k], scalar=wr,
                                                             in1=Wi[:, k:2 * k], op0=ALU.mult, op1=ALU.add))
        rho_bc = RHO[:, Pp:Pp + 1].to_broadcast([128, TOK])
        for sgn in (ALU.subtract, ALU.add):
            s1, s2 = (ALU.subtract, ALU.add) if sgn == ALU.subtract else (ALU.add, ALU.subtract)
            P.dve(lambda e: e.tensor_tensor(out=T1, in0=Wr, in1=Sr, op=ALU.mult), reads=[BW, BSr], writes=BT1)
            P.dve(lambda e: e.tensor_tensor(out=T2, in0=Wi, in1=Si, op=ALU.mult), reads=[BW, BSi], writes=[BT2])
            P.dve(lambda e, s1=s1: e.tensor_tensor(out=T1, in0=T1, in1=T2, op=s1), reads=[BT2], writes=BT1)
            P.dve(lambda e: e.tensor_tensor(out=T2, in0=Wi, in1=Sr, op=ALU.mult), reads=[BW, BSr], writes=[BT2])
            P.dve(lambda e: e.tensor_tensor(out=Si, in0=Wr, in1=Si, op=ALU.mult), reads=[BW], writes=[BSi])
            P.dve(lambda e, s2=s2: e.tensor_tensor(out=Si, in0=Si, in1=T2, op=s2), reads=[BT2], writes=[BSi])
            if sgn == ALU.subtract:
                P.dve(lambda e, rho_bc=rho_bc, Pp=Pp: e.tensor_tensor_scan(out=Sr, data0=rho_bc, data1=T1, initial=INI[:, 0, Pp:Pp + 1],
                                                                           op0=ALU.mult, op1=ALU.add),
                      reads=BT1 + [Bpar], writes=[BSr])
                P.dve(lambda e, rho_bc=rho_bc, Pp=Pp: e.tensor_tensor_scan(out=Si, data0=rho_bc, data1=Si, initial=INI[:, 1, Pp:Pp + 1],
                                                                           op0=ALU.mult, op1=ALU.add),
                      reads=[Bpar], writes=[BSi])
        P.dve(lambda e, Pp=Pp: e.tensor_copy(out=Fsb[:, Pp, 0:1], in_=T1[:, TOK - 1:TOK]), reads=BT1, writes=[BF_])
        P.dve(lambda e, Pp=Pp: e.tensor_copy(out=Fsb[:, Pp, 1:2], in_=Si[:, TOK - 1:TOK]), reads=[BSi], writes=[BF_])
        if mode != "full":
            continue
        P.act(lambda e: e.copy(out=Xb[:, 0, :], in_=T1), reads=BT1, writes=[BXb, Bw[0], Bw[1]])
        P.act(lambda e: e.copy(out=Xb[:, 1, :], in_=Si), reads=[BSi], writes=[BXb])
        yb = 0
        for tg in range(8):
            i = cnt["pb"] % 4
            cnt["pb"] += 1
            cs = slice(tg * 512, (tg + 1) * 512)

            def ymm(e, i=i, Pp=Pp, cs=cs):
                e.matmul(pb[i][:], lhsT=Cblk[:, Pp, 0, :], rhs=Xb[:, 0, cs], start=True, stop=False)
                return e.matmul(pb[i][:], lhsT=Cblk[:, Pp, 1, :], rhs=Xb[:, 1, cs], start=False, stop=True)
            P.pe(ymm, reads=[BCblk, BXb], writes=[Bpb[i]])
            if q == 0:
                P.dve(lambda e, i=i, cs=cs, gq=gq, yb=yb: e.scalar_tensor_tensor(
                    out=stg_f[yb][:, cs], in0=uT[:, gq, cs], scalar=dskt[:, gq:gq + 1], in1=pb[i][:],
                    op0=ALU.mult, op1=ALU.add), reads=[Bpb[i], BuT, Bpar], writes=[Bsf[yb]])
            else:
                P.dve(lambda e, i=i, cs=cs, yb=yb: e.tensor_tensor(
                    out=stg_f[yb][:, cs], in0=stg_f[yb][:, cs], in1=pb[i][:], op=ALU.add),
                    reads=[Bpb[i]], writes=[Bsf[yb]])
        if q == 3:
            outs.append(P.dma("sp", ylocT[gq * 128:(gq + 1) * 128, :], stg_f[yb], reads=[Bsf[yb]]))
    outs.append(P.dma("sp", Fo, Fsb[:], reads=[BF_]))
    if DBG:
        outs.append(P.dma("sp", dbg1, LK[:], reads=[Bpar]))
        outs.append(P.dma("sp", dbg2, tB[:], reads=[Bpar]))
        outs.append(P.dma("sp", dbg3, tP[:], reads=[Bpar]))
        outs.append(P.dma("sp", dbg4, sPt[:], reads=[Bpar]))
    P.fence("sp", outs)
    _finish(P)
    return nc


def _ssm_layouts(inp, l):
    f = lambda k: np.asarray(inp[k][l], np.float32)
    lam_re, lam_im, logdt = f("ssm_lam_re"), f("ssm_lam_im"), f("ssm_log_dt")
    b_re, b_im, c_re, c_im, d = f("ssm_b_re"), f("ssm_b_im"), f("ssm_c_re"), f("ssm_c_im"), f("ssm_d")
    logdt_b = np.broadcast_to(logdt[:, None], (32, 64))

    def Blay_gp(a):
        t = np.transpose(np.asarray(a).reshape(4, 8, 64), (1, 0, 2))
        return np.repeat(t[:, None], 16, axis=1).reshape(128, 4, 64)

    def Blay_b(b):
        return np.transpose(b.reshape(4, 8, 64, 16), (1, 3, 0, 2)).reshape(128, 4, 64)

    def Play(a):
        return np.transpose(np.asarray(a).reshape(16, 2, 64), (1, 2, 0)).reshape(128, 16)

    def Clay(c):
        return np.transpose(c.reshape(16, 2, 16, 64), (1, 3, 0, 2)).reshape(128, 16, 16)
    ssmB = np.stack([Blay_gp(lam_re), Blay_gp(lam_im), Blay_gp(logdt_b), Blay_b(b_re), Blay_b(b_im)], axis=1)
    ssmP = np.stack([Play(lam_re), Play(lam_im), Play(logdt_b)], axis=1)
    ssmC = np.stack([Clay(c_re), Clay(c_im)], axis=1)
    dsk = d.reshape(4, 128).T
    masks = np.zeros((128, 8), np.float32)
    pg = np.arange(128) // 16
    for q in range(4):
        masks[:, q] = (pg == 2 * q)
        masks[:, 4 + q] = (pg == 2 * q + 1)
    c = np.ascontiguousarray
    return dict(ssmB=c(ssmB, np.float32), ssmP=c(ssmP, np.float32), ssmC=c(ssmC, np.float32),
                dsk=c(dsk, np.float32), masks=masks)


def prep_A(inp, l, xs):
    common = dict(gbc=np.ascontiguousarray(np.broadcast_to(np.asarray(inp["norm1_g"][l], np.float32), (128, D))),
                  w_in=np.ascontiguousarray(inp["w_in"][l], np.float32),
                  ident=np.eye(128, dtype=np.float32))
    common.update(_ssm_layouts(inp, l))
    return [dict(common, x=np.ascontiguousarray(xs[c], np.float32)) for c in range(len(xs))]


def build_B1(nc, stage=99, maxblk=10**9, fz=None):
    ins = {}
    if fz is None:
        for g, (w, r) in enumerate(GROUPS):
            L = TOK // r
            ins["qT%d" % g] = _dram(nc, "qT%d" % g, [512, TOK], BF16, "ExternalInput")
            ins["kTh%d" % g] = _dram(nc, "kTh%d" % g, [512, r, 128 + L], BF16, "ExternalInput")
            ins["vh%d" % g] = _dram(nc, "vh%d" % g, [r, 128 + L, 512], BF16, "ExternalInput")
        hv = _dram(nc, "hv", [128, 1], F32, "ExternalInput")
    biasT = _dram(nc, "biasT", [128, 24, 2, 128], F32, "ExternalInput")
    maskT = _dram(nc, "maskT", [128, 2, 128], F32, "ExternalInput")
    oacc = _dram(nc, "oacc", [3, TOK, 520], F32, "ExternalOutput")
    P = _prog(nc)
    qs = P.sb("qs", [128, 4, TOK], BF16)
    ks = P.sb("ks", [128, 4, TOK + 128 * 16], BF16)
    vs = P.sb("vs", [128, 48, 8, 65], BF16)
    EB = P.sb("EB", [128, 24, 2, 128], F32)
    EB0 = P.sb("EB0", [128, 24, 128], F32)
    mk = P.sb("mk", [128, 2, 128], F32)
    hvt = P.sb("hvt", [128, 1], F32)
    pt = [P.sb("pt%d" % i, [128, 4, 2, 128], F32) for i in range(2)]
    ptb = [P.sb("ptb%d" % i, [128, 4, 2, 128], BF16) for i in range(2)]
    ostg = [P.sb("ostg%d" % i, [128, 8, 65], F32) for i in range(2)]
    ps_s = [P.ps("pss%d" % i, [128, 4, 2, 128]) for i in range(2)]
    ps_o = [P.ps("pso%d" % i, [128, 4, 128]) for i in range(2)]
    Bq, Bk, BEB = Buf(), Buf(), Buf()
    Bvt = [Buf() for _ in range(48)]
    Bpt = [Buf(), Buf()]; Bptb = [Buf(), Buf()]; Bos = [Buf(), Buf()]; Bpss = [Buf(), Buf()]; Bpso = [Buf(), Buf()]
    outs = []
    P.dma("act", EB[:], biasT, writes=[BEB])
    P.dma("act", mk[:], maskT, writes=[BEB])
    if fz is None:
        P.dma("act", hvt[:], hv, writes=[BEB])
    else:
        P.dma("act", hvt[:], fz["hv"], writes=[BEB])
    if stage >= 1:
        P.act(lambda e: e.activation(out=EB[:], in_=EB[:], func=AF.Exp), writes=[BEB])
    for t in range(2 if stage >= 2 else 0):
        P.dve(lambda e, t=t: e.tensor_tensor(out=EB[:, :, t, :], in0=EB[:, :, t, :],
                                             in1=mk[:, t, :].unsqueeze(1).to_broadcast([128, 24, 128]), op=ALU.mult),
              writes=[BEB])
    if stage >= 3:
        P.dve(lambda e: e.tensor_scalar(out=EB0[:], in0=EB[:, :, 0, :], scalar1=hvt[:, 0:1], scalar2=None, op0=ALU.mult),
              writes=[BEB])
        P.dve(lambda e: e.memset(vs[:, :, :, 64:65], 1.0), writes=Bvt)
    it = 0
    nblk = 0
    for g, (w, r) in enumerate(GROUPS):
        L = TOK // r
        nb = L // 128
        ntile = r * (nb + 1)
        kv = ks[:, :, 0:r * (128 + L)].rearrange("p k (c m) -> p k c m", c=r)
        if stage < 4:
            continue
        if fz is None:
            P.dma("sp", qs[:], ins["qT%d" % g].rearrange("(k p) t -> p k t", p=128), writes=[Bq])
            P.dma("sp", kv, ins["kTh%d" % g].rearrange("(k p) c m -> p k c m", p=128), writes=[Bk])
            for c in range(r):
                for t in range(nb + 1):
                    P.dma("pool", vs[:, c * (nb + 1) + t, :, 0:64],
                          ins["vh%d" % g][c, t * 128:(t + 1) * 128, :].rearrange("p (j d) -> p j d", d=64),
                          writes=[Bvt[c * (nb + 1) + t]])
        else:
            P.dma("sp", qs[:], fz["qT"][g].rearrange("(k p) t -> p k t", p=128), writes=[Bq])
            if fz["kT_prev"] is None:
                P.dve(lambda e, kv=kv: e.memset(kv[:, :, :, 0:128], 0.0), writes=[Bk])
            for kc in range(4):
                P.dma("sp", kv[:, kc, :, 128:], fz["kT"][g, kc * 128:(kc + 1) * 128, :].rearrange("p (c m) -> p c m", c=r),
                      writes=[Bk])
                if fz["kT_prev"] is not None:
                    P.dma("sp", kv[:, kc, :, 0:128],
                          fz["kT_prev"][g, kc * 128:(kc + 1) * 128, :].rearrange("p (c m) -> p c m", c=r)[:, :, L - 128:L],
                          writes=[Bk])
            for c in range(r):
                for t in range(nb + 1):
                    dstv = vs[:, c * (nb + 1) + t, :, 0:64]
                    if t == 0:
                        if fz["v_prev"] is None:
                            P.dve(lambda e, dstv=dstv: e.memset(dstv, 0.0), writes=[Bvt[c * (nb + 1) + t]])
                            continue
                        srcv = fz["v_prev"][g, c * L + L - 128:c * L + L, :]
                    else:
                        srcv = fz["v"][g, c * L + (t - 1) * 128:c * L + t * 128, :]
                    P.dma("pool", dstv, srcv.rearrange("p (j d) -> p j d", d=64), writes=[Bvt[c * (nb + 1) + t]])
        items = [(c, n, hq) for c in range(r) for n in range(nb) for hq in range(2)]

        def emit_smm(idx, kv=kv, L=L):
            c, n, hq = items[idx]
            b = idx % 2
            q0 = c * L + n * 128

            def smm(e):
                for jj in range(4):
                    j = hq * 4 + jj
                    pr = slice(64 * (j % 2), 64 * (j % 2) + 64)
                    sl = (jj % 2) * 2 + jj // 2
                    for t in range(2):
                        ins_ = e.matmul(ps_s[b][:, sl, t, :], lhsT=kv[pr, j // 2, c, (n + t) * 128:(n + t + 1) * 128],
                                        rhs=qs[pr, j // 2, q0:q0 + 128], start=True, stop=True)
                return ins_
            P.pe(smm, reads=[Bq, Bk], writes=[Bpss[b]])

        if items:
            emit_smm(0)
        for idx, (c, n, hq) in enumerate(items):
            b = idx % 2
            os_ = (idx // 2) % 2
            P.act(lambda e, b=b: e.activation(out=pt[b][:], in_=ps_s[b][:], func=AF.Exp, scale=0.125),
                  reads=[Bpss[b]], writes=[Bpt[b]])
            hs = slice(hq * 4, hq * 4 + 4)
            hsg = slice(8 * g + hq * 4, 8 * g + hq * 4 + 4)
            if n == 0:
                def mul0(e, b=b, hs=hsg):
                    e.tensor_tensor(out=ptb[b][:, :, 0, :], in0=pt[b][:, :, 0, :], in1=EB0[:, hs, :], op=ALU.mult)
                    return e.tensor_tensor(out=ptb[b][:, :, 1, :], in0=pt[b][:, :, 1, :], in1=EB[:, hs, 1, :], op=ALU.mult)
                P.dve(mul0, reads=[Bpt[b], BEB], writes=[Bptb[b]])
            else:
                P.dve(lambda e, b=b, hs=hsg: e.tensor_tensor(out=ptb[b][:], in0=pt[b][:], in1=EB[:, hs, :, :], op=ALU.mult),
                      reads=[Bpt[b], BEB], writes=[Bptb[b]])
            if idx + 1 < len(items):
                emit_smm(idx + 1)

            def pv(e, b=b, hq=hq, c=c, n=n, nb=nb):
                for jj in range(4):
                    j = hq * 4 + jj
                    sl = (jj % 2) * 2 + jj // 2
                    for t in range(2):
                        ins_ = e.matmul(ps_o[b][:, jj, 0:65], lhsT=ptb[b][:, sl, t, :],
                                        rhs=vs[:, c * (nb + 1) + n + t, j, :], start=(t == 0), stop=(t == 1))
                return ins_
            P.pe(pv, reads=[Bptb[b], Bvt[c * (nb + 1) + n], Bvt[c * (nb + 1) + n + 1]], writes=[Bpso[b]])
            P.act(lambda e, b=b, hs=hs, os_=os_: e.copy(out=ostg[os_][:, hs, :], in_=ps_o[b][:, :, 0:65]),
                  reads=[Bpso[b]], writes=[Bos[os_]])
            if hq == 1:
                t0 = c + r * n * 128
                dst = oacc[g, t0:t0 + r * 127 + 1:r, :]
                outs.append(P.dma("sp", dst, ostg[os_][:].rearrange("p j d -> p (j d)"), reads=[Bos[os_]]))
    P.fence("sp", outs)
    _finish(P)
    return nc


def build_B2(nc, moe, stage=99, fprev=None, carry=True):
    NE = 8 if moe else 1
    FF = 3584 if moe else 2816
    NF = FF // 128
    x = _dram(nc, "x", [TOK, D], F32, "ExternalInput")
    oacc = _dram(nc, "oacc", [3, TOK, 520], F32, "ExternalInput")
    sgT = _dram(nc, "sgT", [2048, TOK], F32, "ExternalInput")
    ylocT = _dram(nc, "ylocT", [512, TOK], F32, "ExternalInput")
    Fprev = _dram(nc, "Fprev", [128, 3, 16, 2], F32, "ExternalInput") if fprev is None else None
    ssmP = _dram(nc, "ssmP", [128, 3, 16], F32, "ExternalInput")
    ssmC = _dram(nc, "ssmC", [128, 2, 16, 16], F32, "ExternalInput")
    ident = _dram(nc, "ident", [128, 128], F32, "ExternalInput")
    w_glu = _dram(nc, "w_glu", [512, 512], F32, "ExternalInput")
    bglu = _dram(nc, "bglu", [128, 4], F32, "ExternalInput")
    wab = _dram(nc, "wab", [512, D], F32, "ExternalInput")
    wsb = _dram(nc, "wsb", [512, D], F32, "ExternalInput")
    wo = _dram(nc, "wo", [D, D], F32, "ExternalInput")
    g2bc = _dram(nc, "g2bc", [128, D], F32, "ExternalInput")
    gfbc = _dram(nc, "gfbc", [128, D], F32, "ExternalInput")
    wg = _dram(nc, "wg", [NE, D, FF], F32, "ExternalInput")
    wu = _dram(nc, "wu", [NE, D, FF], F32, "ExternalInput")
    wd = _dram(nc, "wd", [NE, FF, D], F32, "ExternalInput")
    if moe:
        wr = _dram(nc, "wr", [D, 8], F32, "ExternalInput")
    yT = _dram(nc, "yT_scr", [512, TOK], F32, "Internal")
    out = _dram(nc, "out", [TOK, D], F32, "ExternalOutput")

    P = _prog(nc)
    wglu_s = P.sb("wglu_s", [128, 4, 512], BF16)
    wab_s = P.sb("wab_s", [128, 4, D], BF16)
    wsb_s = P.sb("wsb_s", [128, 4, D], BF16)
    wo_s = P.sb("wo_s", [128, 8, D], BF16)
    bglu_s = P.sb("bglu_s", [128, 4], F32)
    g2_s = P.sb("g2_s", [128, D], F32)
    gf_s = P.sb("gf_s", [128, D], F32)
    idf = P.sb("idf", [128, 128], F32)
    idb = P.sb("idb", [128, 128], BF16)
    sPt = P.sb("sPt", [128, 3, 16], F32)
    tP = P.sb("tP", [128, 8, 16], F32)
    LK = P.sb("LK", [128, 3, 13, 16], F32)
    sCt = P.sb("sCt", [128, 2, 16, 16], F32)
    Cblk = P.sb("Cblk", [128, 16, 2, 128], BF16)
    Fp = P.sb("Fp", [128, 3, 16, 2], F32)
    Xin = P.sb("Xin", [128, 4, 16], F32)
    wdt_full = P.sb("wdt", [128, 32, 512], BF16)
    wdt = wdt_full[:, 0:NF, :]
    wgu4 = [[P.sb("wgu%d%d" % (i, j), [128, 8, 256], BF16)[:] for j in range(2)] for i in range(2)]
    actT = P.sb("actT", [128, NF, 512], BF16)
    xm = P.sb("xm", [128, 4, D], F32)
    h2T = P.sb("h2T", [128, 8, 512], BF16)
    h2f = P.sb("h2f", [128, 8, 128], F32)
    ytile = P.sb("ytile", [128, 4, 512], F32)
    ytmp = P.sb("ytmp", [128, 4, 512], F32)
    junk = ytmp[:, 0:2, :].rearrange("p a t -> p (a t)")
    xs = ytmp[:, 2:4, :].rearrange("p a t -> p (a t)")
    zb = P.sb("zb", [128, 4, 512], BF16)
    zz = P.sb("zz", [128, 4, 512], BF16)
    oa = P.sb("oa", [128, 3, 520], F32)
    onb = P.sb("onb", [128, 8, 64], BF16)
    aoT = P.sb("aoT", [128, 4, 512], BF16)
    sgt = P.sb("sgt", [128, 2, 512], F32)
    mtmp = P.sb("mtmp", [128, 512], F32)
    mT = ytile[:].rearrange("p a t -> p (a t)").bitcast(BF16).rearrange("p (k t) -> p k t", k=8)
    small = P.sb("small", [128, 16], F32)
    gat = P.sb("gat", [128, 4, 8], F32)
    lg = P.sb("lg", [128, 4, 8], F32)
    if moe:
        wr_s = P.sb("wr_s", [128, 8, 8], F32)
    Gf = wdt_full[:].rearrange("p f c -> p (f c)").bitcast(F32)[:, 0:8192].rearrange("p (h t) -> p h t", h=2)
    Gb = actT[:, 0:16, :].rearrange("p a t -> p (a t)").rearrange("p (h t) -> p h t", h=2)
    ych = xm[:].rearrange("p a d -> p (a d)")
    pb = [P.ps("pb%d" % i, [128, 512]) for i in range(4)]
    pacc = [P.ps("pacc%d" % i, [128, 512]) for i in range(2)]
    ptp = P.ps("ptp", [128, 8, 128])
    pacc4 = [pacc[0][:], pacc[1][:], ptp[:, 0:4, :].rearrange("p k t -> p (k t)"), ptp[:, 4:8, :].rearrange("p k t -> p (k t)")]
    qb = [0, (NF + 3) // 4, (NF + 3) // 4 + (NF + 2) // 4, (NF + 3) // 4 + (NF + 2) // 4 + (NF + 1) // 4, NF]
    Bw = Buf(); Bpar = Buf(); BC = Buf(); BG = Buf(); BGb = Buf(); Bych = Buf(); ByT = Buf()
    Bpb = [Buf() for _ in range(4)]; Bpacc = [Buf(), Buf()]; Bptp = Buf()
    Bpacc4 = [Bpacc[0], Bpacc[1], Bptp, Bptp]
    Bwdt = Buf(); Bwgu4 = [[Buf(), Buf()], [Buf(), Buf()]]; Bwdq = [Buf() for _ in range(4)]; Bact = Buf(); Bxm = Buf(); Bh2T = Buf(); Bh2f = Buf()
    Byt = Buf(); Bytmp = Buf(); Bzb = Buf(); Bzz = Buf(); Boa = Buf(); Bon = Buf(); BaoT = Buf(); Bsg = Buf()
    Bmt = Buf(); BmT = Byt; Bsm = Buf(); Bgat = Buf(); Bjunk = Bytmp; Bxs = Bytmp
    outs = []
    cnt = {"pb": 0, "gu": 0}

    def nxt():
        i = cnt["pb"] % 4
        cnt["pb"] += 1
        return i

    P.dma("pool", wglu_s[:], w_glu.rearrange("(k p) c -> p k c", p=128), writes=[Bw])
    P.dma("pool", wab_s[:], wab.rearrange("(k p) c -> p k c", p=128), writes=[Bw])
    P.dma("pool", wsb_s[:], wsb.rearrange("(k p) c -> p k c", p=128), writes=[Bw])
    P.dma("pool", wo_s[:], wo.rearrange("(k p) c -> p k c", p=128), writes=[Bw])
    P.dma("pool", idb[:], ident, writes=[Bw])
    for dst, src in ((bglu_s[:], bglu), (g2_s[:], g2bc), (gf_s[:], gfbc), (idf[:], ident), (sPt[:], ssmP),
                     (sCt[:], ssmC)):
        P.dma("act", dst, src, writes=[Bpar])
    if not carry:
        pass
    elif fprev is None:
        P.dma("act", Fp[:], Fprev, writes=[Bpar])
    else:
        P.dve(lambda e: e.memset(Fp[:], 0.0), writes=[Bpar])
        for d_, fa in enumerate(fprev):
            if fa is not None:
                P.dma("act", Fp[:, d_], fa, writes=[Bpar])
    if moe:
        P.dma("act", wr_s[:], wr.rearrange("(k p) e -> p k e", p=128), writes=[Bpar])
    rb, wbp = [Bpar], [Bpar]
    dv = (lambda f: P.dve(f, reads=rb, writes=wbp)) if carry else (lambda f: None)
    TP = lambda i: tP[:, i]
    if carry:
      emit_lb(P, sPt[:, 0], sPt[:, 1], sPt[:, 2], [TP(0), TP(1), TP(2), TP(3)], LK[:, 0, 0], LK[:, 1, 0], rb, wbp)
    for k in range(12):
        dv(lambda e, k=k: e.tensor_tensor(out=TP(0), in0=LK[:, 0, k], in1=LK[:, 0, k], op=ALU.mult))
        dv(lambda e, k=k: e.tensor_tensor(out=TP(1), in0=LK[:, 1, k], in1=LK[:, 1, k], op=ALU.mult))
        dv(lambda e, k=k: e.tensor_tensor(out=LK[:, 0, k + 1], in0=TP(0), in1=TP(1), op=ALU.subtract))
        dv(lambda e, k=k: e.scalar_tensor_tensor(out=LK[:, 1, k + 1], in0=LK[:, 0, k], scalar=2.0, in1=LK[:, 1, k],
                                                  op0=ALU.mult, op1=ALU.mult))
    dv(lambda e: e.tensor_scalar(out=LK[:, 2], in0=LK[:, 1], scalar1=-1.0, scalar2=None, op0=ALU.mult))

    def cmul(o_r, o_i, a_r, a_i, b_r, b_i):
        dv(lambda e: e.tensor_tensor(out=TP(4), in0=a_r, in1=b_r, op=ALU.mult))
        dv(lambda e: e.tensor_tensor(out=TP(5), in0=a_i, in1=b_i, op=ALU.mult))
        dv(lambda e: e.tensor_tensor(out=TP(6), in0=a_r, in1=b_i, op=ALU.mult))
        dv(lambda e: e.tensor_tensor(out=TP(7), in0=a_i, in1=b_r, op=ALU.mult))
        dv(lambda e: e.tensor_tensor(out=o_r, in0=TP(4), in1=TP(5), op=ALU.subtract))
        dv(lambda e: e.tensor_tensor(out=o_i, in0=TP(6), in1=TP(7), op=ALU.add))
    Xr, Xi, G0r, G0i = Xin[:, 0], Xin[:, 1], Xin[:, 2], Xin[:, 3]
    L4r, L4i = LK[:, 0, 12], LK[:, 1, 12]
    cmul(Xr, Xi, Fp[:, 2, :, 0], Fp[:, 2, :, 1], L4r, L4i)
    dv(lambda e: e.tensor_tensor(out=Xr, in0=Xr, in1=Fp[:, 1, :, 0], op=ALU.add))
    dv(lambda e: e.tensor_tensor(out=Xi, in0=Xi, in1=Fp[:, 1, :, 1], op=ALU.add))
    cmul(G0r, G0i, Xr, Xi, L4r, L4i)
    dv(lambda e: e.tensor_tensor(out=Xr, in0=G0r, in1=Fp[:, 0, :, 0], op=ALU.add))
    dv(lambda e: e.tensor_tensor(out=Xi, in0=G0i, in1=Fp[:, 0, :, 1], op=ALU.add))
    cmul(G0r, G0i, Xr, Xi, LK[:, 0, 0], LK[:, 1, 0])
    if carry:
        P.dve(lambda e: e.memset(Cblk[:], 0.0), writes=[BC])
    for Pp in range(16 if carry else 0):
        q = Pp % 4
        for g2 in range(2):
            c0 = 32 * q + 16 * g2
            ps_ = slice(64 * g2, 64 * g2 + 64)
            P.dve(lambda e, Pp=Pp, c0=c0, ps_=ps_: e.tensor_copy(out=Cblk[ps_, Pp, 0, c0:c0 + 16], in_=sCt[ps_, 0, Pp, :]),
                  reads=rb, writes=[BC])
            P.dve(lambda e, Pp=Pp, c0=c0, ps_=ps_: e.tensor_scalar(
                out=Cblk[ps_, Pp, 1, c0:c0 + 16], in0=sCt[ps_, 1, Pp, :], scalar1=-1.0, scalar2=None, op0=ALU.mult),
                reads=rb, writes=[BC])
    for Pp in range(16 if (stage >= 1 and carry) else 0):
        gq, q = Pp // 4, Pp % 4
        gw = lambda f: P.dve(f, reads=[Bpar], writes=[BG] + Bwdq)
        gw(lambda e, Pp=Pp: e.tensor_copy(out=Gf[:, 0, 0:1], in_=G0r[:, Pp:Pp + 1]))
        gw(lambda e, Pp=Pp: e.tensor_copy(out=Gf[:, 1, 0:1], in_=G0i[:, Pp:Pp + 1]))
        for s in range(12):
            k = 1 << s
            lr, li, nli = LK[:, 0, s, Pp:Pp + 1], LK[:, 1, s, Pp:Pp + 1], LK[:, 2, s, Pp:Pp + 1]
            gw(lambda e, k=k, lr=lr: e.tensor_scalar(out=Gf[:, 0, k:2 * k], in0=Gf[:, 0, 0:k], scalar1=lr, scalar2=None,
                                                     op0=ALU.mult))
            gw(lambda e, k=k, nli=nli: e.scalar_tensor_tensor(out=Gf[:, 0, k:2 * k], in0=Gf[:, 1, 0:k], scalar=nli,
                                                              in1=Gf[:, 0, k:2 * k], op0=ALU.mult, op1=ALU.add))
            gw(lambda e, k=k, li=li: e.tensor_scalar(out=Gf[:, 1, k:2 * k], in0=Gf[:, 0, 0:k], scalar1=li, scalar2=None,
                                                     op0=ALU.mult))
            gw(lambda e, k=k, lr=lr: e.scalar_tensor_tensor(out=Gf[:, 1, k:2 * k], in0=Gf[:, 1, 0:k], scalar=lr,
                                                            in1=Gf[:, 1, k:2 * k], op0=ALU.mult, op1=ALU.add))
        P.act(lambda e: e.copy(out=Gb, in_=Gf), reads=[BG] + Bwdq, writes=[BGb, Bact])
        if q == 0:
            P.dma("sp", ych, ylocT[gq * 128:(gq + 1) * 128, :], writes=[Bych, Bxm])
        for tg in range(8):
            i = nxt()
            cs = slice(tg * 512, (tg + 1) * 512)

            def ymm(e, i=i, Pp=Pp, cs=cs):
                e.matmul(pb[i][:], lhsT=Cblk[:, Pp, 0, :], rhs=Gb[:, 0, cs], start=True, stop=False)
                return e.matmul(pb[i][:], lhsT=Cblk[:, Pp, 1, :], rhs=Gb[:, 1, cs], start=False, stop=True)
            P.pe(ymm, reads=[BC, BGb], writes=[Bpb[i]])
            P.dve(lambda e, i=i, cs=cs: e.tensor_tensor(out=ych[:, cs], in0=ych[:, cs], in1=pb[i][:], op=ALU.add),
                  reads=[Bpb[i]], writes=[Bych, Bxm])
        if q == 3:
            P.dma("sp", yT[gq * 128:(gq + 1) * 128, :], ych, reads=[Bych], writes=[ByT])

    for tg in range(8 if stage >= 2 else 0):
        ts_ = slice(tg * 512, (tg + 1) * 512)
        P.dma("sp", ytile[:], (yT if carry else ylocT)[:, ts_].rearrange("(k p) t -> p k t", p=128), reads=[ByT], writes=[Byt])
        P.dve(lambda e: e.tensor_tensor(out=ytmp[:], in0=ytile[:], in1=ytile[:], op=ALU.mult), reads=[Byt], writes=[Bytmp])
        P.dve(lambda e: e.tensor_scalar(out=ytmp[:], in0=ytmp[:], scalar1=0.044715, scalar2=1.0, op0=ALU.mult, op1=ALU.add),
              writes=[Bytmp])
        P.dve(lambda e: e.tensor_tensor(out=ytmp[:], in0=ytmp[:], in1=ytile[:], op=ALU.mult), reads=[Byt], writes=[Bytmp])
        P.act(lambda e: e.activation(out=ytmp[:], in_=ytmp[:], func=AF.Sigmoid, scale=1.5957691216), writes=[Bytmp])
        P.dve(lambda e: e.tensor_tensor(out=zb[:], in0=ytmp[:], in1=ytile[:], op=ALU.mult), reads=[Byt, Bytmp], writes=[Bzb])
        for oc in range(4):
            i = nxt()

            def glu(e, i=i, oc=oc):
                for kc in range(4):
                    ins_ = e.matmul(pb[i][:], lhsT=wglu_s[:, kc, oc * 128:(oc + 1) * 128], rhs=zb[:, kc, :],
                                    start=(kc == 0), stop=(kc == 3))
                return ins_
            P.pe(glu, reads=[Bw, Bzb], writes=[Bpb[i]])
            P.act(lambda e, i=i, oc=oc: e.activation(out=ytmp[:, oc, :], in_=pb[i][:], func=AF.Sigmoid,
                                                     bias=bglu_s[:, oc:oc + 1]), reads=[Bpb[i], Bpar], writes=[Bytmp])
        P.dve(lambda e: e.tensor_tensor(out=zz[:], in0=zb[:], in1=ytmp[:], op=ALU.mult), reads=[Bzb, Bytmp], writes=[Bzz])
        if stage < 3:
            continue
        for tl in range(4):
            t0 = tg * 512 + tl * 128
            P.dma("act", oa[:], oacc[:, t0:t0 + 128, :].rearrange("g p f -> p g f"), writes=[Boa])
            P.dve(lambda e: e.tensor_tensor(out=oa[:, 0, :], in0=oa[:, 0, :], in1=oa[:, 1, :], op=ALU.add), writes=[Boa])
            P.dve(lambda e: e.tensor_tensor(out=oa[:, 0, :], in0=oa[:, 0, :], in1=oa[:, 2, :], op=ALU.add), writes=[Boa])
            U = oa[:, 0, :].rearrange("p (j d) -> p j d", d=65)
            P.dve(lambda e, U=U: e.reciprocal(out=small[:, 0:8], in_=U[:, :, 64]), reads=[Boa], writes=[Bsm])
            P.dve(lambda e, U=U: e.tensor_tensor(out=onb[:], in0=U[:, :, 0:64],
                                                 in1=small[:, 0:8].unsqueeze(2).to_broadcast([128, 8, 64]), op=ALU.mult),
                  reads=[Boa, Bsm], writes=[Bon])
            onf = onb[:].rearrange("p j d -> p (j d)")
            ptb_ = ptp[:].rearrange("p k t -> p (k t)").bitcast(BF16)[:, 0:512].rearrange("p (k t) -> p k t", k=4)

            def otr(e, onf=onf, ptb_=ptb_):
                for kc in range(4):
                    ins_ = e.transpose(out=ptb_[:, kc, :], in_=onf[:, kc * 128:(kc + 1) * 128], identity=idb[:])
                return ins_
            P.pe(otr, reads=[Bon, Bw], writes=[Bptp])
            P.act(lambda e, tl=tl, ptb_=ptb_: e.copy(out=aoT[:, :, tl * 128:(tl + 1) * 128], in_=ptb_),
                  reads=[Bptp], writes=[BaoT])
        if stage < 4:
            continue
        for oc in range(8):
            P.dma("act", sgt[:], sgT[:, ts_].rearrange("(a k p) t -> p a k t", a=2, p=128)[:, :, oc, :], writes=[Bsg])
            ia, ib = nxt(), nxt()

            def br(e, ia=ia, ib=ib, oc=oc):
                for kc in range(4):
                    e.matmul(pb[ia][:], lhsT=wab_s[:, kc, oc * 128:(oc + 1) * 128], rhs=aoT[:, kc, :],
                             start=(kc == 0), stop=(kc == 3))
                for kc in range(4):
                    ins_ = e.matmul(pb[ib][:], lhsT=wsb_s[:, kc, oc * 128:(oc + 1) * 128], rhs=zz[:, kc, :],
                                    start=(kc == 0), stop=(kc == 3))
                return ins_
            P.pe(br, reads=[Bw, BaoT, Bzz], writes=[Bpb[ia], Bpb[ib]])
            P.dve(lambda e, ia=ia: e.tensor_tensor(out=mtmp[:], in0=pb[ia][:], in1=sgt[:, 0, :], op=ALU.mult),
                  reads=[Bpb[ia], Bsg], writes=[Bmt])
            P.dve(lambda e, ib=ib: e.tensor_tensor(out=sgt[:, 1, :], in0=pb[ib][:], in1=sgt[:, 1, :], op=ALU.mult),
                  reads=[Bpb[ib]], writes=[Bsg])
            P.dve(lambda e, oc=oc: e.tensor_tensor(out=mT[:, oc, :], in0=mtmp[:], in1=sgt[:, 1, :], op=ALU.add),
                  reads=[Bmt, Bsg], writes=[BmT])
        if stage < 5:
            continue
        for tl in range(4):
            t0 = tg * 512 + tl * 128
            P.dma("sp", xm[:, tl, :], x[t0:t0 + 128, :], writes=[Bxm])
            for hf in range(2):
                i = nxt()

                def om(e, i=i, tl=tl, hf=hf):
                    for kc in range(8):
                        ins_ = e.matmul(pb[i][:], lhsT=mT[:, kc, tl * 128:(tl + 1) * 128],
                                        rhs=wo_s[:, kc, hf * 512:(hf + 1) * 512], start=(kc == 0), stop=(kc == 7))
                    return ins_
                P.pe(om, reads=[BmT, Bw], writes=[Bpb[i]])
                P.dve(lambda e, i=i, tl=tl, hf=hf: e.tensor_tensor(out=xm[:, tl, hf * 512:(hf + 1) * 512],
                                                                   in0=xm[:, tl, hf * 512:(hf + 1) * 512], in1=pb[i][:],
                                                                   op=ALU.add), reads=[Bpb[i]], writes=[Bxm])
            P.act(lambda e, tl=tl: e.activation(out=junk, in_=xm[:, tl, :], func=AF.Square), reads=[Bxm], writes=[Bjunk])
            P.dve(lambda e: e.reduce_sum(out=small[:, 8:9], in_=junk, axis=AX.X), reads=[Bjunk], writes=[Bsm])
            P.act(lambda e: e.activation(out=small[:, 9:10], in_=small[:, 8:9], func=AF.Sqrt, bias=EPS, scale=1.0 / D),
                  writes=[Bsm])
            P.dve(lambda e: e.reciprocal(out=small[:, 9:10], in_=small[:, 9:10]), writes=[Bsm])
            P.dve(lambda e, tl=tl: e.scalar_tensor_tensor(out=xs, in0=xm[:, tl, :], scalar=small[:, 9:10], in1=g2_s[:],
                                                          op0=ALU.mult, op1=ALU.mult), reads=[Bxm, Bsm, Bpar], writes=[Bxs])

            def tr(e):
                for kc in range(8):
                    ins_ = e.transpose(out=ptp[:, kc, :], in_=xs[:, kc * 128:(kc + 1) * 128], identity=idf[:])
                return ins_
            P.pe(tr, reads=[Bxs, Bpar], writes=[Bptp])
            P.act(lambda e, tl=tl: e.copy(out=h2T[:, :, tl * 128:(tl + 1) * 128], in_=ptp[:]), reads=[Bptp], writes=[Bh2T])
            if moe:
                P.dve(lambda e: e.tensor_copy(out=h2f[:], in_=ptp[:]), reads=[Bptp, Bh2T], writes=[Bh2f])
                if stage < 5.2:
                    continue
                i = nxt()

                def rt(e, i=i):
                    for kc in range(8):
                        ins_ = e.matmul(pb[i][:, 0:8], lhsT=h2f[:, kc, :], rhs=wr_s[:, kc, :], start=(kc == 0), stop=(kc == 7))
                    return ins_
                P.pe(rt, reads=[Bh2f, Bpar], writes=[Bpb[i]])
                if stage < 5.3:
                    continue
                L_ = lg[:, tl, :]
                G_ = gat[:, tl, :]
                s_ = lambda a: small[:, a:a + 1]
                gd = lambda f, rd=(): P.dve(f, reads=list(rd), writes=[Bgat])
                gd(lambda e, i=i, L_=L_: e.tensor_copy(out=L_, in_=pb[i][:, 0:8]), [Bpb[i]])
                gd(lambda e, L_=L_: e.reduce_max(out=s_(10), in_=L_, axis=AX.X))
                gd(lambda e, L_=L_, G_=G_: e.tensor_scalar(out=G_, in0=L_, scalar1=s_(10), scalar2=None, op0=ALU.is_equal))
                gd(lambda e, L_=L_, G_=G_: e.scalar_tensor_tensor(out=L_, in0=G_, scalar=-1e30, in1=L_, op0=ALU.mult, op1=ALU.add))
                if stage < 5.4:
                    continue
                gd(lambda e, L_=L_: e.reduce_max(out=s_(11), in_=L_, axis=AX.X))
                gd(lambda e, L_=L_: e.tensor_scalar(out=L_, in0=L_, scalar1=s_(11), scalar2=None, op0=ALU.is_equal))
                gd(lambda e: e.tensor_tensor(out=s_(12), in0=s_(11), in1=s_(10), op=ALU.subtract))
                if stage < 5.5:
                    continue
                P.act(lambda e: e.activation(out=s_(12), in_=s_(12), func=AF.Exp), writes=[Bgat])
                gd(lambda e: e.tensor_scalar(out=s_(13), in0=s_(12), scalar1=1.0, scalar2=None, op0=ALU.add))
                gd(lambda e: e.reciprocal(out=s_(13), in_=s_(13)))
                gd(lambda e: e.tensor_tensor(out=s_(14), in0=s_(12), in1=s_(13), op=ALU.mult))
                gd(lambda e, G_=G_: e.tensor_scalar(out=G_, in0=G_, scalar1=s_(13), scalar2=None, op0=ALU.mult))
                gd(lambda e, L_=L_, G_=G_: e.scalar_tensor_tensor(out=G_, in0=L_, scalar=s_(14), in1=G_, op0=ALU.mult, op1=ALU.add))
        if stage < 6:
            continue
        for ex in range(NE):
            for f0 in range(0, NF, 2):
                wset = cnt["gu"] % 2
                cnt["gu"] += 1
                wgs, wus = wgu4[wset][0], wgu4[wset][1]
                P.dma("pool", wgs, wg[ex, :, f0 * 128:(f0 + 2) * 128].rearrange("(k p) c -> p k c", p=128),
                      writes=[Bwgu4[wset][0]])
                P.dma("pool", wus, wu[ex, :, f0 * 128:(f0 + 2) * 128].rearrange("(k p) c -> p k c", p=128),
                      writes=[Bwgu4[wset][1]])
                for fc in range(2):
                    ig, iu = nxt(), nxt()

                    def gu(e, ig=ig, iu=iu, fc=fc, wgs=wgs, wus=wus):
                        for kc in range(8):
                            e.matmul(pb[ig][:], lhsT=wgs[:, kc, fc * 128:(fc + 1) * 128], rhs=h2T[:, kc, :],
                                     start=(kc == 0), stop=(kc == 7))
                        for kc in range(8):
                            ins_ = e.matmul(pb[iu][:], lhsT=wus[:, kc, fc * 128:(fc + 1) * 128], rhs=h2T[:, kc, :],
                                            start=(kc == 0), stop=(kc == 7))
                        return ins_
                    P.pe(gu, reads=[Bwgu4[wset][0], Bwgu4[wset][1], Bh2T], writes=[Bpb[ig], Bpb[iu]])
                    P.act(lambda e, ig=ig: e.activation(out=mtmp[:], in_=pb[ig][:], func=AF.Silu), reads=[Bpb[ig]], writes=[Bmt])
                    P.dve(lambda e, iu=iu, f=f0 + fc: e.tensor_tensor(out=actT[:, f, :], in0=mtmp[:], in1=pb[iu][:], op=ALU.mult),
                          reads=[Bmt, Bpb[iu]], writes=[Bact])
            for hf in range(2):
                for qi in range(4):
                    fa, fb = qb[qi], qb[qi + 1]
                    wq = wdt_full[:, 8 * qi:8 * qi + (fb - fa), :]
                    P.dma("pool", wq, wd[ex, fa * 128:fb * 128, hf * 512:(hf + 1) * 512].rearrange("(f p) c -> p f c", p=128),
                          writes=[Bwdq[qi]])
                    for tl in range(4):
                        def dn(e, tl=tl, fa=fa, fb=fb, wq=wq):
                            for f in range(fa, fb):
                                ins_ = e.matmul(pacc4[tl], lhsT=actT[:, f, tl * 128:(tl + 1) * 128], rhs=wq[:, f - fa, :],
                                                start=(f == 0), stop=(f == NF - 1))
                            return ins_
                        P.pe(dn, reads=[Bact, Bwdq[qi]], writes=[Bpacc4[tl]])
                for tl in range(4):
                    xsl = xm[:, tl, hf * 512:(hf + 1) * 512]
                    if moe:
                        P.dve(lambda e, xsl=xsl, tl=tl, ex=ex: e.scalar_tensor_tensor(
                            out=xsl, in0=pacc4[tl], scalar=gat[:, tl, ex:ex + 1], in1=xsl, op0=ALU.mult, op1=ALU.add),
                            reads=[Bpacc4[tl], Bgat], writes=[Bxm])
                    else:
                        P.dve(lambda e, xsl=xsl, tl=tl: e.tensor_tensor(out=xsl, in0=xsl, in1=pacc4[tl], op=ALU.add),
                              reads=[Bpacc4[tl]], writes=[Bxm])
        if stage < 7:
            continue
        for tl in range(4):
            t0 = tg * 512 + tl * 128
            if moe:
                P.act(lambda e, tl=tl: e.activation(out=junk, in_=xm[:, tl, :], func=AF.Square), reads=[Bxm], writes=[Bjunk])
                P.dve(lambda e: e.reduce_sum(out=small[:, 8:9], in_=junk, axis=AX.X), reads=[Bjunk], writes=[Bsm])
                P.act(lambda e: e.activation(out=small[:, 9:10], in_=small[:, 8:9], func=AF.Sqrt, bias=EPS, scale=1.0 / D),
                      writes=[Bsm])
                P.dve(lambda e: e.reciprocal(out=small[:, 9:10], in_=small[:, 9:10]), writes=[Bsm])
                P.dve(lambda e, tl=tl: e.scalar_tensor_tensor(out=xs, in0=xm[:, tl, :], scalar=small[:, 9:10], in1=gf_s[:],
                                                              op0=ALU.mult, op1=ALU.mult), reads=[Bxm, Bsm, Bpar], writes=[Bxs])
                outs.append(P.dma("sp", out[t0:t0 + 128, :], xs, reads=[Bxs]))
            else:
                outs.append(P.dma("sp", out[t0:t0 + 128, :], xm[:, tl, :], reads=[Bxm]))
    P.fence("sp", outs)
    _finish(P)
    return nc


def _t5_bucket(dist):
    max_exact = 16
    d = np.maximum(dist, 1).astype(np.float64)
    large = max_exact + (np.log(d / max_exact) / np.log(2048 / max_exact) * (32 - max_exact)).astype(np.int32)
    large = np.minimum(large, 31)
    return np.where(dist < max_exact, dist, large).astype(np.int32)


def prep_B1_tables(inp):
    rel = np.asarray(inp["rel_bias"], np.float32)
    kk = np.arange(128)[:, None]
    qi = np.arange(128)[None, :]
    biasT = np.zeros((128, 24, 2, 128), np.float32)
    maskT = np.zeros((128, 2, 128), np.float32)
    for t in range(2):
        delta = (128 if t == 0 else 0) + qi - kk
        maskT[:, t, :] = ((delta >= 0) & (delta <= 128)).astype(np.float32)
        for g, (w, r) in enumerate(GROUPS):
            bucket = _t5_bucket(np.clip(delta, 0, 128) * r)
            hp = 8 * g + np.array([0, 2, 1, 3, 4, 6, 5, 7])
            biasT[:, 8 * g:8 * g + 8, t, :] = np.transpose(rel[bucket][:, :, hp], (0, 2, 1))
    return dict(biasT=biasT, maskT=maskT)


def prep_B1(inp, resA):
    tb = prep_B1_tables(inp)
    biasT, maskT = tb["biasT"], tb["maskT"]
    maps = []
    for c in range(8):
        ci = c % 4
        m = dict(biasT=biasT, maskT=maskT, hv=np.full((128, 1), 1.0 if ci > 0 else 0.0, np.float32))
        for g, (w, r) in enumerate(GROUPS):
            L = TOK // r
            own_k = np.asarray(resA[c]["kT"][g]).reshape(512, r, L)
            own_v = np.asarray(resA[c]["v"][g]).reshape(r, L, 512)
            if ci > 0:
                hk = np.asarray(resA[c - 1]["kT"][g]).reshape(512, r, L)[:, :, L - 128:]
                hvv = np.asarray(resA[c - 1]["v"][g]).reshape(r, L, 512)[:, L - 128:, :]
            else:
                hk = np.zeros((512, r, 128), own_k.dtype)
                hvv = np.zeros((r, 128, 512), own_v.dtype)
            m["qT%d" % g] = np.ascontiguousarray(resA[c]["qT"][g])
            m["kTh%d" % g] = np.ascontiguousarray(np.concatenate([hk, own_k], axis=2))
            m["vh%d" % g] = np.ascontiguousarray(np.concatenate([hvv, own_v], axis=1))
        maps.append(m)
    return maps


def prep_B2(inp, l, xs, resA, resB1):
    f = lambda a: np.ascontiguousarray(a, np.float32)
    lay = _ssm_layouts(inp, l)
    common = dict(ssmP=lay["ssmP"], ssmC=lay["ssmC"], ident=np.eye(128, dtype=np.float32),
                  w_glu=f(inp["w_glu"][l]), bglu=f(np.asarray(inp["b_glu"][l]).reshape(4, 128).T),
                  wab=f(inp["w_attn_br"][l]), wsb=f(inp["w_ssm_br"][l]), wo=f(inp["w_out"][l]),
                  g2bc=f(np.broadcast_to(np.asarray(inp["norm2_g"][l]), (128, D))),
                  gfbc=f(np.broadcast_to(np.asarray(inp["final_norm_g"]), (128, D))))
    if l % 2 == 0:
        i = l // 2
        common.update(wg=f(inp["ffn_w_gate"][i][None]), wu=f(inp["ffn_w_up"][i][None]), wd=f(inp["ffn_w_down"][i][None]))
    else:
        i = l // 2
        common.update(wg=f(inp["moe_w_gate"][i]), wu=f(inp["moe_w_up"][i]), wd=f(inp["moe_w_down"][i]),
                      wr=f(inp["moe_router"][i]))
    maps = []
    for c in range(8):
        ci = c % 4
        Fprev = np.zeros((128, 3, 16, 2), np.float32)
        for d in range(3):
            if ci - 1 - d >= 0:
                Fprev[:, d] = resA[c - 1 - d]["F"]
        maps.append(dict(common, x=f(xs[c]), oacc=f(resB1[c]["oacc"]), sgT=f(resA[c]["sgT"]),
                         ylocT=f(resA[c]["ylocT"]), Fprev=Fprev))
    return maps


NCH = 4
SEQ = NCH * TOK
ARENA_BYTES = 212800


def build_fused(nc):
    global _OVR, _PROG
    I = lambda name, shape, dt=F32: nc.dram_tensor(name, list(shape), dt, kind="ExternalInput").ap()
    S = lambda name, shape, dt=F32: nc.dram_tensor(name, list(shape), dt, kind="Internal").ap()
    x = I("x", [SEQ, D])
    hvv = I("hvv", [NCH, 128, 1])
    out = nc.dram_tensor("out", [TOK, D], F32, kind="ExternalOutput").ap()
    com = dict(ident=I("ident", [128, 128]), masks=I("masks", [128, 8]), biasT=I("biasT", [128, 24, 2, 128]),
               maskT=I("maskT", [128, 2, 128]), gfbc=I("gfbc", [128, D]))
    lay = []
    for l in range(2):
        lay.append(dict(gbc=I("gbc%d" % l, [128, D]), w_in=I("w_in%d" % l, [D, 7168]),
                        ssmB=I("ssmB%d" % l, [128, 5, 4, 64]), ssmP=I("ssmP%d" % l, [128, 3, 16]),
                        ssmC=I("ssmC%d" % l, [128, 2, 16, 16]), dsk=I("dsk%d" % l, [128, 4]),
                        w_glu=I("w_glu%d" % l, [512, 512]), bglu=I("bglu%d" % l, [128, 4]),
                        wab=I("wab%d" % l, [512, D]), wsb=I("wsb%d" % l, [512, D]), wo=I("wo%d" % l, [D, D]),
                        g2bc=I("g2bc%d" % l, [128, D])))
    ffn = [dict(wg=I("wg0", [1, D, 2816]), wu=I("wu0", [1, D, 2816]), wd=I("wd0", [1, 2816, D])),
           dict(wg=I("wg1", [8, D, 3584]), wu=I("wu1", [8, D, 3584]), wd=I("wd1", [8, 3584, D]), wr=I("wr", [D, 8]))]
    x1 = S("x1_scr", [SEQ, D])
    ck = []
    for k in range(NCH):
        ck.append(dict(qT=S("qT_%d" % k, [3, 512, TOK], BF16), kT=S("kT_%d" % k, [3, 512, TOK], BF16),
                       v=S("v_%d" % k, [3, TOK, 512], BF16), sgT=S("sgT_%d" % k, [2048, TOK]),
                       ylocT=S("ylocT_%d" % k, [512, TOK]), F=S("F_%d" % k, [128, 16, 2]),
                       oacc=S("oacc_%d" % k, [3, TOK, 520]), yT_scr=S("yT_%d" % k, [512, TOK])))
    P = Prog(nc, arena_bytes=ARENA_BYTES)
    _PROG = P
    try:
        for l in range(2):
            xin = x if l == 0 else x1
            L_ = lay[l]
            own = list(range(NCH)) if l == 0 else [NCH - 1]
            for k in range(NCH):
                c = ck[k]
                _OVR = dict(com, x=xin[k * TOK:(k + 1) * TOK, :], gbc=L_["gbc"], w_in=L_["w_in"], ssmB=L_["ssmB"],
                            ssmP=L_["ssmP"], ssmC=L_["ssmC"], dsk=L_["dsk"], qT=c["qT"], kT=c["kT"], v=c["v"],
                            sgT=c["sgT"], ylocT=c["ylocT"], F=c["F"])
                if k > 0:
                    _OVR["Fin"] = ck[k - 1]["F"]
                build_A(nc, mode="full" if (l == 0 or k == NCH - 1) else ("kvu" if k == NCH - 2 else "u"), prev=(k > 0))
            for k in own:
                c = ck[k]
                p = ck[k - 1] if k > 0 else None
                _OVR = dict(com, oacc=c["oacc"])
                build_B1(nc, fz=dict(qT=c["qT"], kT=c["kT"], v=c["v"], kT_prev=p["kT"] if p else None,
                                     v_prev=p["v"] if p else None, hv=hvv[k]))
            for k in own:
                c = ck[k]
                xo = x1[k * TOK:(k + 1) * TOK, :] if l == 0 else out
                _OVR = dict(com, x=xin[k * TOK:(k + 1) * TOK, :], out=xo, oacc=c["oacc"],
                            sgT=c["sgT"], ylocT=c["ylocT"], yT_scr=c["yT_scr"], ssmP=L_["ssmP"], ssmC=L_["ssmC"],
                            w_glu=L_["w_glu"], bglu=L_["bglu"], wab=L_["wab"], wsb=L_["wsb"], wo=L_["wo"],
                            g2bc=L_["g2bc"], **ffn[l])
                build_B2(nc, moe=(l == 1), fprev=[None, None, None], carry=False)
        P.emit()
        P.close()
    finally:
        _OVR = None
        _PROG = None
    return nc


def prep_fused(inp):
    f = lambda a: np.ascontiguousarray(a, np.float32)
    bc = lambda a: f(np.broadcast_to(np.asarray(a, np.float32), (128, D)))
    b1 = prep_B1_tables(inp)
    com = dict(ident=np.eye(128, dtype=np.float32), biasT=b1["biasT"], maskT=b1["maskT"], gfbc=bc(inp["final_norm_g"]),
               wg0=f(inp["ffn_w_gate"][0][None]), wu0=f(inp["ffn_w_up"][0][None]), wd0=f(inp["ffn_w_down"][0][None]),
               wg1=f(inp["moe_w_gate"][0]), wu1=f(inp["moe_w_up"][0]), wd1=f(inp["moe_w_down"][0]),
               wr=f(inp["moe_router"][0]))
    for l in range(2):
        lay = _ssm_layouts(inp, l)
        com["masks"] = lay["masks"]
        com.update({"gbc%d" % l: bc(inp["norm1_g"][l]), "w_in%d" % l: f(inp["w_in"][l]), "ssmB%d" % l: lay["ssmB"],
                    "ssmP%d" % l: lay["ssmP"], "ssmC%d" % l: lay["ssmC"], "dsk%d" % l: lay["dsk"],
                    "w_glu%d" % l: f(inp["w_glu"][l]), "bglu%d" % l: f(np.asarray(inp["b_glu"][l]).reshape(4, 128).T),
                    "wab%d" % l: f(inp["w_attn_br"][l]), "wsb%d" % l: f(inp["w_ssm_br"][l]), "wo%d" % l: f(inp["w_out"][l]),
                    "g2bc%d" % l: bc(inp["norm2_g"][l])})
    x = np.asarray(inp["x"], np.float32)
    maps = []
    for c in range(8):
        b, k = c // NCH, c % NCH
        xp = np.zeros((SEQ, D), np.float32)
        xp[(NCH - 1 - k) * TOK:] = x[b, :(k + 1) * TOK]
        hvv = np.zeros((NCH, 128, 1), np.float32)
        hvv[NCH - k:] = 1.0
        maps.append(dict(com, x=xp, hvv=hvv))
    return maps


def kernel(**inp):
    nc = bass.Bass("TRN2", target_bir_lowering=False)
    build_fused(nc)
    res = run_bass_kernel_spmd(nc, prep_fused(inp), core_ids=list(range(8))).results
    o = np.stack([np.asarray(r["out"], np.float32) for r in res])
    return o.reshape(2, SEQ, D)
```

```python
import numpy as np
import ml_dtypes
from concourse.bass_utils import run_bass_kernel_spmd
import concourse.bass as bass
import concourse.mybir as mybir
from contextlib import ExitStack

F32 = mybir.dt.float32
BF16 = mybir.dt.bfloat16
I32 = mybir.dt.int32
ALU = mybir.AluOpType
AF = mybir.ActivationFunctionType
AX = mybir.AxisListType

ENGS = ("pe", "act", "dve", "pool", "sp")
NDSEM = 8


class Buf:
    __slots__ = ("name", "last_w", "readers")

    def __init__(self, name=""):
        self.name = name
        self.last_w = None
        self.readers = []


class Op:
    __slots__ = ("eng", "fn", "deps", "is_dma", "signal", "sig_val", "dsem", "dval", "dprev", "idx")

    def __init__(self, eng, fn, deps, is_dma):
        self.eng = eng
        self.fn = fn
        self.deps = deps
        self.is_dma = is_dma
        self.signal = False
        self.sig_val = 0
        self.dsem = None
        self.dval = 0
        self.dprev = 0
        self.idx = 0


class Prog:
    def __init__(self, nc, arena_bytes=0):
        self.nc = nc
        self.q = {e: [] for e in ENGS}
        self.es = ExitStack()
        self.nops = 0
        self.arena = None
        self.phase_dmas = []
        if arena_bytes:
            self.arena = self.es.enter_context(nc.sbuf_tensor("arena_all", [128, arena_bytes // 2], BF16))
            self.parena = self.es.enter_context(nc.psum_tensor("psum_all", [128, 4096], F32))
            self.arena_bytes = arena_bytes
            self.off = 0
            self.poff = 0

    @staticmethod
    def _shape_view(flat, shape):
        if len(shape) == 2:
            return flat
        names = " ".join("d%d" % i for i in range(1, len(shape)))
        kw = {"d%d" % i: int(shape[i]) for i in range(1, len(shape) - 1)}
        return flat.rearrange("p (%s) -> p %s" % (names, names), **kw)

    def sb(self, name, shape, dt):
        if self.arena is None:
            return self.es.enter_context(self.nc.sbuf_tensor(name, list(shape), dt))
        assert shape[0] == 128
        esz = 2 if dt == BF16 else 4
        n = int(np.prod(shape[1:]))
        nbytes = (n * esz + 63) // 64 * 64
        assert self.off + nbytes <= self.arena_bytes, "SBUF arena overflow %s %d" % (name, self.off + nbytes)
        flat = self.arena[:, self.off // 2:self.off // 2 + n * esz // 2]
        self.off += nbytes
        if dt != BF16:
            flat = flat.bitcast(dt)
        return self._shape_view(flat, shape)

    def ps(self, name, shape, dt=F32):
        if self.arena is None:
            return self.es.enter_context(self.nc.psum_tensor(name, list(shape), dt))
        n = int(np.prod(shape[1:]))
        nb = (n + 511) // 512
        assert self.poff + nb * 512 <= 4096, "PSUM arena overflow"
        flat = self.parena[:, self.poff:self.poff + n]
        self.poff += nb * 512
        return self._shape_view(flat, shape)

    def phase_end(self):
        deps = list(self.phase_dmas)
        for e in ENGS:
            for o in reversed(self.q[e]):
                if not o.is_dma and o.fn is not None:
                    deps.append(o)
                    break
        for e in ENGS:
            self.op(e, None, deps=deps)
        self.phase_dmas = []
        self.off = 0
        self.poff = 0

    def op(self, eng, fn, reads=(), writes=(), deps=(), dma=False):
        d = [x for x in deps if x is not None]
        for b in reads:
            if b.last_w is not None:
                d.append(b.last_w)
        for b in writes:
            if b.last_w is not None:
                d.append(b.last_w)
            d.extend(b.readers)
        o = Op(eng, fn, d, dma)
        if dma:
            self.phase_dmas.append(o)
        o.idx = self.nops
        self.nops += 1
        self.q[eng].append(o)
        for b in reads:
            b.readers.append(o)
        for b in writes:
            b.last_w = o
            b.readers = []
        return o

    def pe(self, fn, reads=(), writes=(), deps=()):
        return self.op("pe", fn, reads, writes, deps)

    def act(self, fn, reads=(), writes=(), deps=()):
        return self.op("act", fn, reads, writes, deps)

    def dve(self, fn, reads=(), writes=(), deps=()):
        return self.op("dve", fn, reads, writes, deps)

    def pool(self, fn, reads=(), writes=(), deps=()):
        return self.op("pool", fn, reads, writes, deps)

    def dma(self, eng, out, in_, reads=(), writes=(), deps=(), **kw):
        return self.op(eng, lambda e: e.dma_start(out=out, in_=in_, **kw), reads, writes, deps, dma=True)

    def fence(self, eng, deps):
        return self.op(eng, None, (), (), deps)

    def emit(self):
        nc = self.nc
        for e in ENGS:
            for o in self.q[e]:
                for d in o.deps:
                    if d.is_dma or d.eng != o.eng or o.eng != "pe":
                        d.signal = True
        for e in ENGS:
            cnt = 0
            dcnt = 0
            duse = [0] * NDSEM
            for o in self.q[e]:
                if o.is_dma:
                    k = dcnt % NDSEM
                    dcnt += 1
                    o.dsem = k
                    o.dprev = duse[k]
                    duse[k] += 16
                    o.dval = duse[k]
                elif o.signal:
                    cnt += 1
                    o.sig_val = cnt
        sems = {e: self.es.enter_context(nc.semaphore("s_" + e)) for e in ENGS}
        dsems = {e: [self.es.enter_context(nc.semaphore("d_%s%d" % (e, k))) for k in range(NDSEM)]
                 for e in ("sp", "act", "pool")}
        block = self.es.enter_context(nc.Block())

        def run(ename, eng):
            waited = {}

            def wait(sem, key, val):
                if waited.get(key, 0) >= val:
                    return
                waited[key] = val
                eng.wait_ge(sem, val)

            for o in self.q[ename]:
                for d in o.deps:
                    if d.is_dma:
                        wait(dsems[d.eng][d.dsem], ("d", d.eng, d.dsem), d.dval)
                    elif d.eng != ename or ename != "pe":
                        wait(sems[d.eng], ("e", d.eng), d.sig_val)
                if o.is_dma:
                    if o.dprev:
                        wait(dsems[ename][o.dsem], ("d", ename, o.dsem), o.dprev)
                    ins = o.fn(eng)
                    ins.then_inc(dsems[ename][o.dsem], 16)
                elif o.fn is not None:
                    ins = o.fn(eng)
                    if o.signal:
                        ins.then_inc(sems[ename], 1)

        @block.tensor
        def _(eng):
            run("pe", eng)

        @block.scalar
        def _(eng):
            run("act", eng)

        @block.vector
        def _(eng):
            run("dve", eng)

        @block.gpsimd
        def _(eng):
            run("pool", eng)

        @block.sync
        def _(eng):
            run("sp", eng)

    def close(self):
        self.es.close()


TOK = 4096
D = 1024
EPS = 1e-6
GROUPS = ((128, 1), (512, 4), (2048, 16))
PI = float(np.pi)


def tok_slices(r, n):
    L = TOK // r
    n = min(n, L)
    out = []
    for c in range(r):
        for m0 in range(0, L, n):
            out.append((c * L + m0, slice(c + r * m0, c + r * (m0 + n - 1) + 1, r)))
    return out


_OVR = None
_PROG = None


def _dram(nc, name, shape, dt, kind):
    if _OVR is not None and name in _OVR:
        return _OVR[name]
    assert _OVR is None, "fused mode: missing DRAM binding for " + name
    return nc.dram_tensor(name, list(shape), dt, kind=kind).ap()


def _prog(nc):
    return _PROG if _PROG is not None else Prog(nc)


def _finish(P):
    if _PROG is not None:
        P.phase_end()
    else:
        P.emit()
        P.close()


def emit_lb(P, lamre, lamim, logdt, tmp, LR, LI, rb, wb, rho=None, cs=None):
    t0, t1, t2, t3 = tmp[0], tmp[1], tmp[2], tmp[3]
    dv = lambda f: P.dve(f, reads=rb, writes=wb)
    ac = lambda f: P.act(f, reads=rb, writes=wb)
    ac(lambda e: e.activation(out=t0, in_=logdt, func=AF.Exp))
    dv(lambda e: e.tensor_tensor(out=t1, in0=lamre, in1=t0, op=ALU.mult))
    dv(lambda e: e.tensor_tensor(out=t2, in0=lamim, in1=t0, op=ALU.mult))
    ac(lambda e: e.activation(out=t1, in_=t1, func=AF.Exp))
    dv(lambda e: e.tensor_scalar(out=t0, in0=t2, scalar1=1.0 / (2 * PI), scalar2=None, op0=ALU.mult))
    dv(lambda e: e.tensor_copy(out=t3.bitcast(I32), in_=t0))
    dv(lambda e: e.tensor_copy(out=t0, in_=t3.bitcast(I32)))
    dv(lambda e: e.scalar_tensor_tensor(out=t2, in0=t0, scalar=-2 * PI, in1=t2, op0=ALU.mult, op1=ALU.add))
    ac(lambda e: e.activation(out=t0, in_=t2, func=AF.Sin, scale=0.5))
    ac(lambda e: e.activation(out=t3, in_=t2, func=AF.Sin, scale=0.25))
    dv(lambda e: e.tensor_tensor(out=t2, in0=t0, in1=t0, op=ALU.mult))
    dv(lambda e: e.tensor_scalar(out=t2, in0=t2, scalar1=-2.0, scalar2=1.0, op0=ALU.mult, op1=ALU.add))
    dv(lambda e: e.tensor_tensor(out=t3, in0=t3, in1=t3, op=ALU.mult))
    dv(lambda e: e.tensor_scalar(out=t3, in0=t3, scalar1=-2.0, scalar2=1.0, op0=ALU.mult, op1=ALU.add))
    dv(lambda e: e.scalar_tensor_tensor(out=t3, in0=t0, scalar=2.0, in1=t3, op0=ALU.mult, op1=ALU.mult))
    dv(lambda e: e.tensor_tensor(out=t0, in0=t2, in1=t2, op=ALU.mult))
    dv(lambda e: e.tensor_tensor(out=LR, in0=t3, in1=t3, op=ALU.mult))
    dv(lambda e: e.tensor_tensor(out=LR, in0=t0, in1=LR, op=ALU.add))
    ac(lambda e: e.activation(out=t0, in_=LR, func=AF.Sqrt))
    dv(lambda e: e.reciprocal(out=t0, in_=t0))
    dv(lambda e: e.tensor_tensor(out=LI, in0=t0, in1=t0, op=ALU.mult))
    dv(lambda e: e.tensor_tensor(out=LI, in0=LI, in1=LR, op=ALU.mult))
    dv(lambda e: e.tensor_scalar(out=LI, in0=LI, scalar1=-0.5, scalar2=1.5, op0=ALU.mult, op1=ALU.add))
    dv(lambda e: e.tensor_tensor(out=t0, in0=t0, in1=LI, op=ALU.mult))
    dv(lambda e: e.tensor_tensor(out=t2, in0=t2, in1=t0, op=ALU.mult))
    dv(lambda e: e.tensor_tensor(out=t3, in0=t3, in1=t0, op=ALU.mult))
    if rho is not None:
        dv(lambda e: e.tensor_copy(out=rho, in_=t1))
        dv(lambda e: e.tensor_copy(out=cs[0], in_=t2))
        dv(lambda e: e.tensor_copy(out=cs[1], in_=t3))
    dv(lambda e: e.tensor_tensor(out=LR, in0=t1, in1=t2, op=ALU.mult))
    dv(lambda e: e.tensor_tensor(out=LI, in0=t1, in1=t3, op=ALU.mult))


def build_A(nc, mode="full", prev=False):
    x = _dram(nc, "x", [TOK, D], F32, "ExternalInput")
    gbc = _dram(nc, "gbc", [128, D], F32, "ExternalInput")
    w_in = _dram(nc, "w_in", [D, 7168], F32, "ExternalInput")
    ident = _dram(nc, "ident", [128, 128], F32, "ExternalInput")
    ssmB = _dram(nc, "ssmB", [128, 5, 4, 64], F32, "ExternalInput")
    ssmP = _dram(nc, "ssmP", [128, 3, 16], F32, "ExternalInput")
    ssmC = _dram(nc, "ssmC", [128, 2, 16, 16], F32, "ExternalInput")
    dsk = _dram(nc, "dsk", [128, 4], F32, "ExternalInput")
    masks = _dram(nc, "masks", [128, 8], F32, "ExternalInput")
    qT = _dram(nc, "qT", [3, 512, TOK], BF16, "ExternalOutput")
    kT = _dram(nc, "kT", [3, 512, TOK], BF16, "ExternalOutput")
    vv = _dram(nc, "v", [3, TOK, 512], BF16, "ExternalOutput")
    sgT = _dram(nc, "sgT", [2048, TOK], F32, "ExternalOutput")
    ylocT = _dram(nc, "ylocT", [512, TOK], F32, "ExternalOutput")
    Fo = _dram(nc, "F", [128, 16, 2], F32, "ExternalOutput")
    Fin = _dram(nc, "Fin", [128, 16, 2], F32, "ExternalInput") if prev else None
    DBG = False
    if DBG:
        dbg1 = _dram(nc, "dbg_LK", [128, 3, 1, 16], F32, "ExternalOutput")
        dbg2 = _dram(nc, "dbg_tB", [128, 10, 4, 64], F32, "ExternalOutput")
        dbg3 = _dram(nc, "dbg_tP", [128, 8, 16], F32, "ExternalOutput")
        dbg4 = _dram(nc, "dbg_sP", [128, 3, 16], F32, "ExternalOutput")

    P = _prog(nc)
    arena = P.sb("arena", [128, 32768], BF16)
    hT = arena[:].rearrange("p (k t) -> p k t", k=8)
    scan = arena[:].bitcast(F32).rearrange("p (a h t) -> p a h t", a=2, h=2)
    uT = P.sb("uT", [128, 4, TOK], BF16)
    warena = P.sb("warena", [128, 8192], BF16)
    wbuf = [warena[:, i * 4096:(i + 1) * 4096].rearrange("p (k c) -> p k c", k=8) for i in range(2)]
    Xb = warena[:].rearrange("p (h t) -> p h t", h=2)
    stgbf_t = P.sb("stgbf", [128, 2, TOK], BF16)
    stg_bf = [stgbf_t[:, i, :] for i in range(2)]
    T1 = stgbf_t[:].rearrange("p a t -> p (a t)").bitcast(F32)
    stgf_t = P.sb("stgf", [128, 2, TOK], F32)
    stg_f = [stgf_t[:, i, :] for i in range(2)]
    xt = [stgf_t[:, 0, i * 1024:(i + 1) * 1024] for i in range(2)]
    xs = [stgf_t[:, 0, (2 + i) * 1024:(3 + i) * 1024] for i in range(2)]
    junk = stgf_t[:, 1, 0:1024]
    gb = stgf_t[:, 1, 1024:2048]
    idf = P.sb("idf", [128, 128], F32)
    small = P.sb("small", [128, 8], F32)
    sBt = P.sb("sBt", [128, 5, 4, 64], F32)
    tB = P.sb("tB", [128, 10, 4, 64], F32)
    sPt = P.sb("sPt", [128, 3, 16], F32)
    tP = P.sb("tP", [128, 8, 16], F32)
    LK = P.sb("LK", [128, 3, 1, 16], F32)
    WK = P.sb("WK", [128, 3, 12, 16], F32)
    RHO = P.sb("RHO", [128, 16], F32)
    Fpv = P.sb("Fpv", [128, 16, 2], F32)
    INI = P.sb("INI", [128, 2, 16], F32)
    sCt = P.sb("sCt", [128, 2, 16, 16], F32)
    dskt = P.sb("dskt", [128, 4], F32)
    mskt = P.sb("mskt", [128, 8], F32)
    Bblk = P.sb("Bblk", [128, 16, 2, 128], BF16)
    Cblk = P.sb("Cblk", [128, 16, 2, 128], BF16)
    Fsb = P.sb("Fsb", [128, 16, 2], F32)
    vstg = [P.sb("vstg%d" % i, [128, 4, 512], BF16) for i in range(2)]
    tp = [P.ps("tp%d" % i, [128, 8, 128]) for i in range(2)]
    pb = [P.ps("pb%d" % i, [128, 512]) for i in range(4)]

    Bxt = [Buf(), Buf()]; Bxs = [Buf(), Buf()]; Bsm = [Buf(), Buf()]; Btp = [Buf(), Buf()]
    Bjunk = Buf(); Bgb = Buf(); Bid = Buf(); BhT = [Buf(), Buf()]; Bpb = [Buf() for _ in range(4)]
    Bw = [Buf(), Buf()]; Bsbf = [Buf(), Buf()]; Bsf = [Buf(), Buf()]; BuT = Buf(); Bvs = [Buf(), Buf()]
    Bpar = Buf(); BBblk = Buf(); BCblk = Buf(); BF_ = Buf(); BXb = Buf()
    BA = [[Buf(), Buf()], [Buf(), Buf()]]
    outs = []

    P.dma("act", idf[:], ident, writes=[Bid])
    P.dma("act", gb, gbc, writes=[Bgb])
    P.dma("act", sBt[:], ssmB, writes=[Bpar])
    P.dma("act", sPt[:], ssmP, writes=[Bpar])
    P.dma("act", sCt[:], ssmC, writes=[Bpar])
    P.dma("act", dskt[:], dsk, writes=[Bpar])
    P.dma("act", mskt[:], masks, writes=[Bpar])

    def ph1(tt):
        b = tt % 2
        ss = small[:, b:b + 1]
        rs = small[:, 2 + b:3 + b]
        P.dma("sp", xt[b], x[tt * 128:(tt + 1) * 128, :], writes=[Bxt[b]])
        P.act(lambda e: e.activation(out=junk, in_=xt[b], func=AF.Square), reads=[Bxt[b]], writes=[Bjunk])
        P.dve(lambda e: e.reduce_sum(out=ss, in_=junk, axis=AX.X), reads=[Bjunk], writes=[Bsm[b]])
        P.act(lambda e: e.activation(out=rs, in_=ss, func=AF.Sqrt, bias=EPS, scale=1.0 / D), writes=[Bsm[b]])
        P.dve(lambda e: e.reciprocal(out=rs, in_=rs), writes=[Bsm[b]])
        P.dve(lambda e: e.scalar_tensor_tensor(out=xs[b], in0=xt[b], scalar=rs, in1=gb, op0=ALU.mult, op1=ALU.mult),
              reads=[Bxt[b], Bsm[b], Bgb], writes=[Bxs[b]])

        def tr(e):
            for kc in range(8):
                ins = e.transpose(out=tp[b][:, kc, :], in_=xs[b][:, kc * 128:(kc + 1) * 128], identity=idf[:])
            return ins
        P.pe(tr, reads=[Bxs[b], Bid], writes=[Btp[b]])
        P.act(lambda e: e.copy(out=hT[:, 0:4, tt * 128:(tt + 1) * 128], in_=tp[b][:, 0:4, :]),
              reads=[Btp[b]], writes=[BhT[0]])
        P.dve(lambda e: e.tensor_copy(out=hT[:, 4:8, tt * 128:(tt + 1) * 128], in_=tp[b][:, 4:8, :]),
              reads=[Btp[b]], writes=[BhT[1]])
    for tt in range(TOK // 128):
        ph1(tt)

    cnt = {"pb": 0, "ev": 0, "sb": 0, "sf": 0, "vs": 0}

    def mm_group(lhs_fn, rhs_fn, n):
        i = cnt["pb"] % 4
        cnt["pb"] += 1

        def f(e):
            for kc in range(8):
                ins = e.matmul(pb[i][:, 0:n], lhsT=lhs_fn(kc), rhs=rhs_fn(kc), start=(kc == 0), stop=(kc == 7))
            return ins
        return i, f

    def load_w(cg):
        P.dma("pool", wbuf[cg % 2], w_in[:, cg * 512:(cg + 1) * 512].rearrange("(k p) c -> p k c", p=128),
              writes=[Bw[cg % 2]])

    cgs = {"full": list(range(14)), "kvu": [3, 4, 5, 6, 7, 8, 9], "u": [9]}[mode]
    load_w(cgs[0])
    for ci_, cg in enumerate(cgs):
        if ci_ + 1 < len(cgs):
            load_w(cgs[ci_ + 1])
        wb = wbuf[cg % 2]
        Bwc = Bw[cg % 2]
        if cg in (6, 7, 8):
            g = cg - 6
            r = GROUPS[g][1]
            for ti, (pos0, tsl) in enumerate(tok_slices(r, 128)):
                s = (cnt["vs"] // 4) % 2
                a = cnt["vs"] % 4
                cnt["vs"] += 1
                i, f = mm_group(lambda kc, tsl=tsl: hT[:, kc, tsl], lambda kc, wb=wb: wb[:, kc, :], 512)
                P.pe(f, reads=[BhT[0], BhT[1], Bwc], writes=[Bpb[i]])
                if ti % 2 == 0:
                    P.act(lambda e, i=i, s=s, a=a: e.copy(out=vstg[s][:, a, :], in_=pb[i][:]),
                          reads=[Bpb[i]], writes=[Bvs[s]])
                else:
                    P.dve(lambda e, i=i, s=s, a=a: e.tensor_copy(out=vstg[s][:, a, :], in_=pb[i][:]),
                          reads=[Bpb[i]], writes=[Bvs[s]])
                if a == 3:
                    outs.append(P.dma("sp", vv[g, pos0 - 384:pos0 + 128, :].rearrange("(a p) c -> p a c", p=128),
                                      vstg[s][:], reads=[Bvs[s]]))
            continue
        if cg < 6:
            r = GROUPS[cg % 3][1]
        else:
            r = 1
        for cc in range(4):
            if cg < 6:
                s = cnt["sb"] % 2
                cnt["sb"] += 1
                dst, Bdst = stg_bf[s], Bsbf[s]
            elif cg == 9:
                dst, Bdst = uT[:, cc, :], BuT
            else:
                s = cnt["sf"] % 2
                cnt["sf"] += 1
                dst, Bdst = stg_f[s], Bsf[s]
            for (pos0, tsl) in tok_slices(r, 512):
                n = min(512, TOK // r)
                i, f = mm_group(lambda kc, wb=wb, cc=cc: wb[:, kc, cc * 128:(cc + 1) * 128],
                                lambda kc, tsl=tsl: hT[:, kc, tsl], n)
                P.pe(f, reads=[BhT[0], BhT[1], Bwc], writes=[Bpb[i]])
                o = dst[:, pos0:pos0 + n]
                if cg >= 10:
                    P.act(lambda e, i=i, o=o, n=n: e.activation(out=o, in_=pb[i][:, 0:n], func=AF.Sigmoid),
                          reads=[Bpb[i]], writes=[Bdst])
                else:
                    P.dve(lambda e, i=i, o=o, n=n: e.tensor_copy(out=o, in_=pb[i][:, 0:n]),
                          reads=[Bpb[i]], writes=[Bdst])
            if cg < 3:
                outs.append(P.dma("sp", qT[cg, cc * 128:(cc + 1) * 128, :], dst, reads=[Bdst]))
            elif cg < 6:
                outs.append(P.dma("sp", kT[cg - 3, cc * 128:(cc + 1) * 128, :], dst, reads=[Bdst]))
            elif cg >= 10:
                row = (cg - 10) * 512 + cc * 128
                outs.append(P.dma("sp", sgT[row:row + 128, :], dst, reads=[Bdst]))

    rb, wbp = [Bpar], [Bpar]
    T = lambda i: tB[:, i]
    emit_lb(P, sBt[:, 0], sBt[:, 1], sBt[:, 2], [T(0), T(1), T(2), T(3)], T(4), T(5), rb, wbp)
    LRb, LIb, lre, lim, bre, bim = T(4), T(5), sBt[:, 0], sBt[:, 1], sBt[:, 3], sBt[:, 4]
    dv = lambda f: P.dve(f, reads=rb, writes=wbp)
    dv(lambda e: e.tensor_scalar(out=T(0), in0=LRb, scalar1=-1.0, scalar2=None, op0=ALU.add))
    dv(lambda e: e.tensor_tensor(out=T(1), in0=lre, in1=lre, op=ALU.mult))
    dv(lambda e: e.tensor_tensor(out=T(2), in0=lim, in1=lim, op=ALU.mult))
    dv(lambda e: e.tensor_tensor(out=T(1), in0=T(1), in1=T(2), op=ALU.add))
    dv(lambda e: e.reciprocal(out=T(1), in_=T(1)))
    dv(lambda e: e.tensor_tensor(out=T(2), in0=T(0), in1=lre, op=ALU.mult))
    dv(lambda e: e.tensor_tensor(out=T(3), in0=LIb, in1=lim, op=ALU.mult))
    dv(lambda e: e.tensor_tensor(out=T(2), in0=T(2), in1=T(3), op=ALU.add))
    dv(lambda e: e.tensor_tensor(out=T(3), in0=LIb, in1=lre, op=ALU.mult))
    dv(lambda e: e.tensor_tensor(out=T(6), in0=T(0), in1=lim, op=ALU.mult))
    dv(lambda e: e.tensor_tensor(out=T(3), in0=T(3), in1=T(6), op=ALU.subtract))
    dv(lambda e: e.tensor_tensor(out=T(2), in0=T(2), in1=T(1), op=ALU.mult))
    dv(lambda e: e.tensor_tensor(out=T(3), in0=T(3), in1=T(1), op=ALU.mult))
    dv(lambda e: e.tensor_tensor(out=T(6), in0=T(2), in1=bre, op=ALU.mult))
    dv(lambda e: e.tensor_tensor(out=T(7), in0=T(3), in1=bim, op=ALU.mult))
    dv(lambda e: e.tensor_tensor(out=T(8), in0=T(6), in1=T(7), op=ALU.subtract))
    dv(lambda e: e.tensor_tensor(out=T(6), in0=T(2), in1=bim, op=ALU.mult))
    dv(lambda e: e.tensor_tensor(out=T(7), in0=T(3), in1=bre, op=ALU.mult))
    dv(lambda e: e.tensor_tensor(out=T(9), in0=T(6), in1=T(7), op=ALU.add))
    for Pp in range(16):
        gq, q = Pp // 4, Pp % 4
        for h in range(2):
            src = T(8 + h)[:, gq, :]
            P.dve(lambda e, Pp=Pp, h=h, q=q, src=src: e.tensor_scalar(
                out=Bblk[:, Pp, h, 0:64], in0=src, scalar1=mskt[:, q:q + 1], scalar2=None, op0=ALU.mult),
                reads=rb, writes=[BBblk])
            P.dve(lambda e, Pp=Pp, h=h, q=q, src=src: e.tensor_scalar(
                out=Bblk[:, Pp, h, 64:128], in0=src, scalar1=mskt[:, 4 + q:5 + q], scalar2=None, op0=ALU.mult),
                reads=rb, writes=[BBblk])
    TP = lambda i: tP[:, i]
    emit_lb(P, sPt[:, 0], sPt[:, 1], sPt[:, 2], [TP(0), TP(1), TP(2), TP(3)], LK[:, 0, 0], LK[:, 1, 0], rb, wbp,
            rho=RHO[:], cs=(WK[:, 0, 0], WK[:, 2, 0]))
    dv(lambda e: e.tensor_scalar(out=WK[:, 1, 0], in0=WK[:, 2, 0], scalar1=-1.0, scalar2=None, op0=ALU.mult))
    for k in range(11):
        dv(lambda e, k=k: e.tensor_tensor(out=TP(0), in0=WK[:, 0, k], in1=WK[:, 0, k], op=ALU.mult))
        dv(lambda e, k=k: e.tensor_tensor(out=TP(1), in0=WK[:, 1, k], in1=WK[:, 1, k], op=ALU.mult))
        dv(lambda e, k=k: e.tensor_tensor(out=WK[:, 0, k + 1], in0=TP(0), in1=TP(1), op=ALU.subtract))
        dv(lambda e, k=k: e.scalar_tensor_tensor(out=WK[:, 1, k + 1], in0=WK[:, 0, k], scalar=2.0, in1=WK[:, 1, k],
                                                  op0=ALU.mult, op1=ALU.mult))
    dv(lambda e: e.tensor_scalar(out=WK[:, 2], in0=WK[:, 1], scalar1=-1.0, scalar2=None, op0=ALU.mult))
    if prev:
        P.dma("act", Fpv[:], Fin, writes=[Bpar])
        dv(lambda e: e.tensor_tensor(out=TP(0), in0=WK[:, 0, 0], in1=Fpv[:, :, 0], op=ALU.mult))
        dv(lambda e: e.tensor_tensor(out=TP(1), in0=WK[:, 2, 0], in1=Fpv[:, :, 1], op=ALU.mult))
        dv(lambda e: e.tensor_tensor(out=INI[:, 0], in0=TP(0), in1=TP(1), op=ALU.subtract))
        dv(lambda e: e.tensor_tensor(out=TP(0), in0=WK[:, 2, 0], in1=Fpv[:, :, 0], op=ALU.mult))
        dv(lambda e: e.tensor_tensor(out=TP(1), in0=WK[:, 0, 0], in1=Fpv[:, :, 1], op=ALU.mult))
        dv(lambda e: e.tensor_tensor(out=INI[:, 1], in0=TP(0), in1=TP(1), op=ALU.add))
    else:
        dv(lambda e: e.memset(INI[:], 0.0))
    P.dve(lambda e: e.memset(Cblk[:], 0.0), writes=[BCblk])
    for Pp in range(16):
        q = Pp % 4
        for g2 in range(2):
            c0 = 32 * q + 16 * g2
            ps_ = slice(64 * g2, 64 * g2 + 64)
            P.dve(lambda e, Pp=Pp, c0=c0, ps_=ps_: e.tensor_copy(out=Cblk[ps_, Pp, 0, c0:c0 + 16], in_=sCt[ps_, 0, Pp, :]),
                  reads=rb, writes=[BCblk])
            P.dve(lambda e, Pp=Pp, c0=c0, ps_=ps_: e.tensor_scalar(
                out=Cblk[ps_, Pp, 1, c0:c0 + 16], in0=sCt[ps_, 1, Pp, :], scalar1=-1.0, scalar2=None, op0=ALU.mult),
                reads=rb, writes=[BCblk])

    for Pp in range(16):
        gq, q = Pp // 4, Pp % 4
        for tg in range(8):
            for h in range(2):
                i = cnt["pb"] % 4
                cnt["pb"] += 1
                P.pe(lambda e, i=i, Pp=Pp, h=h, tg=tg, gq=gq: e.matmul(
                    pb[i][:], lhsT=Bblk[:, Pp, h, :], rhs=uT[:, gq, tg * 512:(tg + 1) * 512], start=True, stop=True),
                    reads=[BBblk, BuT], writes=[Bpb[i]])
                P.act(lambda e, i=i, h=h, tg=tg: e.copy(out=scan[:, 0, h, tg * 512:(tg + 1) * 512], in_=pb[i][:]),
                      reads=[Bpb[i]], writes=[BA[0][h]])
        Sr, Si, Wr, Wi = scan[:, 0, 0], scan[:, 0, 1], scan[:, 1, 0], scan[:, 1, 1]
        T2 = stg_f[1]
        BSr, BSi, BW, BT1, BT2 = BA[0][0], BA[0][1], BA[1][0], [Bsbf[0], Bsbf[1]], Bsf[1]
        wd_ = lambda f: P.dve(f, reads=[Bpar], writes=[BW])
        wd_(lambda e: e.memset(Wr[:, 0:1], 1.0))
        wd_(lambda e: e.memset(Wi[:, 0:1], 0.0))
        for s_ in range(12):
            k = 1 << s_
            wr, wi, nwi = WK[:, 0, s_, Pp:Pp + 1], WK[:, 1, s_, Pp:Pp + 1], WK[:, 2, s_, Pp:Pp + 1]
            wd_(lambda e, k=k, wr=wr: e.tensor_scalar(out=Wr[:, k:2 * k], in0=Wr[:, 0:k], scalar1=wr, scalar2=None,
                                                      op0=ALU.mult))
            wd_(lambda e, k=k, nwi=nwi: e.scalar_tensor_tensor(out=Wr[:, k:2 * k], in0=Wi[:, 0:k], scalar=nwi,
                                                               in1=Wr[:, k:2 * k], op0=ALU.mult, op1=ALU.add))
            wd_(lambda e, k=k, wi=wi: e.tensor_scalar(out=Wi[:, k:2 * k], in0=Wr[:, 0:k], scalar1=wi, scalar2=None,
                                                      op0=ALU.mult))
            wd_(lambda e, k=k, wr=wr: e.scalar_tensor_tensor(out=Wi[:, k:2 * k], in0=Wi[:, 0:k], scalar=wr,
                                                             in1=Wi[:, k:2 * k], op0=ALU.mult, op1=ALU.add))
        rho_bc = RHO[:, Pp:Pp + 1].to_broadcast([128, TOK])
        for sgn in (ALU.subtract, ALU.add):
            s1, s2 = (ALU.subtract, ALU.add) if sgn == ALU.subtract else (ALU.add, ALU.subtract)
            P.dve(lambda e: e.tensor_tensor(out=T1, in0=Wr, in1=Sr, op=ALU.mult), reads=[BW, BSr], writes=BT1)
            P.dve(lambda e: e.tensor_tensor(out=T2, in0=Wi, in1=Si, op=ALU.mult), reads=[BW, BSi], writes=[BT2])
            P.dve(lambda e, s1=s1: e.tensor_tensor(out=T1, in0=T1, in1=T2, op=s1), reads=[BT2], writes=BT1)
            P.dve(lambda e: e.tensor_tensor(out=T2, in0=Wi, in1=Sr, op=ALU.mult), reads=[BW, BSr], writes=[BT2])
            P.dve(lambda e: e.tensor_tensor(out=Si, in0=Wr, in1=Si, op=ALU.mult), reads=[BW], writes=[BSi])
            P.dve(lambda e, s2=s2: e.tensor_tensor(out=Si, in0=Si, in1=T2, op=s2), reads=[BT2], writes=[BSi])
            if sgn == ALU.subtract:
                P.dve(lambda e, rho_bc=rho_bc, Pp=Pp: e.tensor_tensor_scan(out=Sr, data0=rho_bc, data1=T1, initial=INI[:, 0, Pp:Pp + 1],
                                                                           op0=ALU.mult, op1=ALU.add),
                      reads=BT1 + [Bpar], writes=[BSr])
                P.dve(lambda e, rho_bc=rho_bc, Pp=Pp: e.tensor_tensor_scan(out=Si, data0=rho_bc, data1=Si, initial=INI[:, 1, Pp:Pp + 1],
                                                                           op0=ALU.mult, op1=ALU.add),
                      reads=[Bpar], writes=[BSi])
        P.dve(lambda e, Pp=Pp: e.tensor_copy(out=Fsb[:, Pp, 0:1], in_=T1[:, TOK - 1:TOK]), reads=BT1, writes=[BF_])
        P.dve(lambda e, Pp=Pp: e.tensor_copy(out=Fsb[:, Pp, 1:2], in_=Si[:, TOK - 1:TOK]), reads=[BSi], writes=[BF_])
        if mode != "full":
            continue
        P.act(lambda e: e.copy(out=Xb[:, 0, :], in_=T1), reads=BT1, writes=[BXb, Bw[0], Bw[1]])
        P.act(lambda e: e.copy(out=Xb[:, 1, :], in_=Si), reads=[BSi], writes=[BXb])
        yb = 0
        for tg in range(8):
            i = cnt["pb"] % 4
            cnt["pb"] += 1
            cs = slice(tg * 512, (tg + 1) * 512)

            def ymm(e, i=i, Pp=Pp, cs=cs):
                e.matmul(pb[i][:], lhsT=Cblk[:, Pp, 0, :], rhs=Xb[:, 0, cs], start=True, stop=False)
                return e.matmul(pb[i][:], lhsT=Cblk[:, Pp, 1, :], rhs=Xb[:, 1, cs], start=False, stop=True)
            P.pe(ymm, reads=[BCblk, BXb], writes=[Bpb[i]])
            if q == 0:
                P.dve(lambda e, i=i, cs=cs, gq=gq, yb=yb: e.scalar_tensor_tensor(
                    out=stg_f[yb][:, cs], in0=uT[:, gq, cs], scalar=dskt[:, gq:gq + 1], in1=pb[i][:],
                    op0=ALU.mult, op1=ALU.add), reads=[Bpb[i], BuT, Bpar], writes=[Bsf[yb]])
            else:
                P.dve(lambda e, i=i, cs=cs, yb=yb: e.tensor_tensor(
                    out=stg_f[yb][:, cs], in0=stg_f[yb][:, cs], in1=pb[i][:], op=ALU.add),
                    reads=[Bpb[i]], writes=[Bsf[yb]])
        if q == 3:
            outs.append(P.dma("sp", ylocT[gq * 128:(gq + 1) * 128, :], stg_f[yb], reads=[Bsf[yb]]))
    outs.append(P.dma("sp", Fo, Fsb[:], reads=[BF_]))
    if DBG:
        outs.append(P.dma("sp", dbg1, LK[:], reads=[Bpar]))
        outs.append(P.dma("sp", dbg2, tB[:], reads=[Bpar]))
        outs.append(P.dma("sp", dbg3, tP[:], reads=[Bpar]))
        outs.append(P.dma("sp", dbg4, sPt[:], reads=[Bpar]))
    P.fence("sp", outs)
    _finish(P)
    return nc


def _ssm_layouts(inp, l):
    f = lambda k: np.asarray(inp[k][l], np.float32)
    lam_re, lam_im, logdt = f("ssm_lam_re"), f("ssm_lam_im"), f("ssm_log_dt")
    b_re, b_im, c_re, c_im, d = f("ssm_b_re"), f("ssm_b_im"), f("ssm_c_re"), f("ssm_c_im"), f("ssm_d")
    logdt_b = np.broadcast_to(logdt[:, None], (32, 64))

    def Blay_gp(a):
        t = np.transpose(np.asarray(a).reshape(4, 8, 64), (1, 0, 2))
        return np.repeat(t[:, None], 16, axis=1).reshape(128, 4, 64)

    def Blay_b(b):
        return np.transpose(b.reshape(4, 8, 64, 16), (1, 3, 0, 2)).reshape(128, 4, 64)

    def Play(a):
        return np.transpose(np.asarray(a).reshape(16, 2, 64), (1, 2, 0)).reshape(128, 16)

    def Clay(c):
        return np.transpose(c.reshape(16, 2, 16, 64), (1, 3, 0, 2)).reshape(128, 16, 16)
    ssmB = np.stack([Blay_gp(lam_re), Blay_gp(lam_im), Blay_gp(logdt_b), Blay_b(b_re), Blay_b(b_im)], axis=1)
    ssmP = np.stack([Play(lam_re), Play(lam_im), Play(logdt_b)], axis=1)
    ssmC = np.stack([Clay(c_re), Clay(c_im)], axis=1)
    dsk = d.reshape(4, 128).T
    masks = np.zeros((128, 8), np.float32)
    pg = np.arange(128) // 16
    for q in range(4):
        masks[:, q] = (pg == 2 * q)
        masks[:, 4 + q] = (pg == 2 * q + 1)
    c = np.ascontiguousarray
    return dict(ssmB=c(ssmB, np.float32), ssmP=c(ssmP, np.float32), ssmC=c(ssmC, np.float32),
                dsk=c(dsk, np.float32), masks=masks)


def prep_A(inp, l, xs):
    common = dict(gbc=np.ascontiguousarray(np.broadcast_to(np.asarray(inp["norm1_g"][l], np.float32), (128, D))),
                  w_in=np.ascontiguousarray(inp["w_in"][l], np.float32),
                  ident=np.eye(128, dtype=np.float32))
    common.update(_ssm_layouts(inp, l))
    return [dict(common, x=np.ascontiguousarray(xs[c], np.float32)) for c in range(len(xs))]


def build_B1(nc, stage=99, maxblk=10**9, fz=None):
    ins = {}
    if fz is None:
        for g, (w, r) in enumerate(GROUPS):
            L = TOK // r
            ins["qT%d" % g] = _dram(nc, "qT%d" % g, [512, TOK], BF16, "ExternalInput")
            ins["kTh%d" % g] = _dram(nc, "kTh%d" % g, [512, r, 128 + L], BF16, "ExternalInput")
            ins["vh%d" % g] = _dram(nc, "vh%d" % g, [r, 128 + L, 512], BF16, "ExternalInput")
        hv = _dram(nc, "hv", [128, 1], F32, "ExternalInput")
    biasT = _dram(nc, "biasT", [128, 24, 2, 128], F32, "ExternalInput")
    maskT = _dram(nc, "maskT", [128, 2, 128], F32, "ExternalInput")
    oacc = _dram(nc, "oacc", [3, TOK, 520], F32, "ExternalOutput")
    P = _prog(nc)
    qs = P.sb("qs", [128, 4, TOK], BF16)
    ks = P.sb("ks", [128, 4, TOK + 128 * 16], BF16)
    vs = P.sb("vs", [128, 48, 8, 65], BF16)
    EB = P.sb("EB", [128, 24, 2, 128], F32)
    EB0 = P.sb("EB0", [128, 24, 128], F32)
    mk = P.sb("mk", [128, 2, 128], F32)
    hvt = P.sb("hvt", [128, 1], F32)
    pt = [P.sb("pt%d" % i, [128, 4, 2, 128], F32) for i in range(2)]
    ptb = [P.sb("ptb%d" % i, [128, 4, 2, 128], BF16) for i in range(2)]
    ostg = [P.sb("ostg%d" % i, [128, 8, 65], F32) for i in range(2)]
    ps_s = [P.ps("pss%d" % i, [128, 4, 2, 128]) for i in range(2)]
    ps_o = [P.ps("pso%d" % i, [128, 4, 128]) for i in range(2)]
    Bq, Bk, BEB = Buf(), Buf(), Buf()
    Bvt = [Buf() for _ in range(48)]
    Bpt = [Buf(), Buf()]; Bptb = [Buf(), Buf()]; Bos = [Buf(), Buf()]; Bpss = [Buf(), Buf()]; Bpso = [Buf(), Buf()]
    outs = []
    P.dma("act", EB[:], biasT, writes=[BEB])
    P.dma("act", mk[:], maskT, writes=[BEB])
    if fz is None:
        P.dma("act", hvt[:], hv, writes=[BEB])
    else:
        P.dma("act", hvt[:], fz["hv"], writes=[BEB])
    if stage >= 1:
        P.act(lambda e: e.activation(out=EB[:], in_=EB[:], func=AF.Exp), writes=[BEB])
    for t in range(2 if stage >= 2 else 0):
        P.dve(lambda e, t=t: e.tensor_tensor(out=EB[:, :, t, :], in0=EB[:, :, t, :],
                                             in1=mk[:, t, :].unsqueeze(1).to_broadcast([128, 24, 128]), op=ALU.mult),
              writes=[BEB])
    if stage >= 3:
        P.dve(lambda e: e.tensor_scalar(out=EB0[:], in0=EB[:, :, 0, :], scalar1=hvt[:, 0:1], scalar2=None, op0=ALU.mult),
              writes=[BEB])
        P.dve(lambda e: e.memset(vs[:, :, :, 64:65], 1.0), writes=Bvt)
    it = 0
    nblk = 0
    for g, (w, r) in enumerate(GROUPS):
        L = TOK // r
        nb = L // 128
        ntile = r * (nb + 1)
        kv = ks[:, :, 0:r * (128 + L)].rearrange("p k (c m) -> p k c m", c=r)
        if stage < 4:
            continue
        if fz is None:
            P.dma("sp", qs[:], ins["qT%d" % g].rearrange("(k p) t -> p k t", p=128), writes=[Bq])
            P.dma("sp", kv, ins["kTh%d" % g].rearrange("(k p) c m -> p k c m", p=128), writes=[Bk])
            for c in range(r):
                for t in range(nb + 1):
                    P.dma("pool", vs[:, c * (nb + 1) + t, :, 0:64],
                          ins["vh%d" % g][c, t * 128:(t + 1) * 128, :].rearrange("p (j d) -> p j d", d=64),
                          writes=[Bvt[c * (nb + 1) + t]])
        else:
            P.dma("sp", qs[:], fz["qT"][g].rearrange("(k p) t -> p k t", p=128), writes=[Bq])
            if fz["kT_prev"] is None:
                P.dve(lambda e, kv=kv: e.memset(kv[:, :, :, 0:128], 0.0), writes=[Bk])
            for kc in range(4):
                P.dma("sp", kv[:, kc, :, 128:], fz["kT"][g, kc * 128:(kc + 1) * 128, :].rearrange("p (c m) -> p c m", c=r),
                      writes=[Bk])
                if fz["kT_prev"] is not None:
                    P.dma("sp", kv[:, kc, :, 0:128],
                          fz["kT_prev"][g, kc * 128:(kc + 1) * 128, :].rearrange("p (c m) -> p c m", c=r)[:, :, L - 128:L],
                          writes=[Bk])
            for c in range(r):
                for t in range(nb + 1):
                    dstv = vs[:, c * (nb + 1) + t, :, 0:64]
                    if t == 0:
                        if fz["v_prev"] is None:
                            P.dve(lambda e, dstv=dstv: e.memset(dstv, 0.0), writes=[Bvt[c * (nb + 1) + t]])
                            continue
                        srcv = fz["v_prev"][g, c * L + L - 128:c * L + L, :]
                    else:
                        srcv = fz["v"][g, c * L + (t - 1) * 128:c * L + t * 128, :]
                    P.dma("pool", dstv, srcv.rearrange("p (j d) -> p j d", d=64), writes=[Bvt[c * (nb + 1) + t]])
        items = [(c, n, hq) for c in range(r) for n in range(nb) for hq in range(2)]

        def emit_smm(idx, kv=kv, L=L):
            c, n, hq = items[idx]
            b = idx % 2
            q0 = c * L + n * 128

            def smm(e):
                for jj in range(4):
                    j = hq * 4 + jj
                    pr = slice(64 * (j % 2), 64 * (j % 2) + 64)
                    sl = (jj % 2) * 2 + jj // 2
                    for t in range(2):
                        ins_ = e.matmul(ps_s[b][:, sl, t, :], lhsT=kv[pr, j // 2, c, (n + t) * 128:(n + t + 1) * 128],
                                        rhs=qs[pr, j // 2, q0:q0 + 128], start=True, stop=True)
                return ins_
            P.pe(smm, reads=[Bq, Bk], writes=[Bpss[b]])

        if items:
            emit_smm(0)
        for idx, (c, n, hq) in enumerate(items):
            b = idx % 2
            os_ = (idx // 2) % 2
            P.act(lambda e, b=b: e.activation(out=pt[b][:], in_=ps_s[b][:], func=AF.Exp, scale=0.125),
                  reads=[Bpss[b]], writes=[Bpt[b]])
            hs = slice(hq * 4, hq * 4 + 4)
            hsg = slice(8 * g + hq * 4, 8 * g + hq * 4 + 4)
            if n == 0:
                def mul0(e, b=b, hs=hsg):
                    e.tensor_tensor(out=ptb[b][:, :, 0, :], in0=pt[b][:, :, 0, :], in1=EB0[:, hs, :], op=ALU.mult)
                    return e.tensor_tensor(out=ptb[b][:, :, 1, :], in0=pt[b][:, :, 1, :], in1=EB[:, hs, 1, :], op=ALU.mult)
                P.dve(mul0, reads=[Bpt[b], BEB], writes=[Bptb[b]])
            else:
                P.dve(lambda e, b=b, hs=hsg: e.tensor_tensor(out=ptb[b][:], in0=pt[b][:], in1=EB[:, hs, :, :], op=ALU.mult),
                      reads=[Bpt[b], BEB], writes=[Bptb[b]])
            if idx + 1 < len(items):
                emit_smm(idx + 1)

            def pv(e, b=b, hq=hq, c=c, n=n, nb=nb):
                for jj in range(4):
                    j = hq * 4 + jj
                    sl = (jj % 2) * 2 + jj // 2
                    for t in range(2):
                        ins_ = e.matmul(ps_o[b][:, jj, 0:65], lhsT=ptb[b][:, sl, t, :],
                                        rhs=vs[:, c * (nb + 1) + n + t, j, :], start=(t == 0), stop=(t == 1))
                return ins_
            P.pe(pv, reads=[Bptb[b], Bvt[c * (nb + 1) + n], Bvt[c * (nb + 1) + n + 1]], writes=[Bpso[b]])
            P.act(lambda e, b=b, hs=hs, os_=os_: e.copy(out=ostg[os_][:, hs, :], in_=ps_o[b][:, :, 0:65]),
                  reads=[Bpso[b]], writes=[Bos[os_]])
            if hq == 1:
                t0 = c + r * n * 128
                dst = oacc[g, t0:t0 + r * 127 + 1:r, :]
                outs.append(P.dma("sp", dst, ostg[os_][:].rearrange("p j d -> p (j d)"), reads=[Bos[os_]]))
    P.fence("sp", outs)
    _finish(P)
    return nc


def build_B2(nc, moe, stage=99, fprev=None, carry=True):
    NE = 8 if moe else 1
    FF = 3584 if moe else 2816
    NF = FF // 128
    x = _dram(nc, "x", [TOK, D], F32, "ExternalInput")
    oacc = _dram(nc, "oacc", [3, TOK, 520], F32, "ExternalInput")
    sgT = _dram(nc, "sgT", [2048, TOK], F32, "ExternalInput")
    ylocT = _dram(nc, "ylocT", [512, TOK], F32, "ExternalInput")
    Fprev = _dram(nc, "Fprev", [128, 3, 16, 2], F32, "ExternalInput") if fprev is None else None
    ssmP = _dram(nc, "ssmP", [128, 3, 16], F32, "ExternalInput")
    ssmC = _dram(nc, "ssmC", [128, 2, 16, 16], F32, "ExternalInput")
    ident = _dram(nc, "ident", [128, 128], F32, "ExternalInput")
    w_glu = _dram(nc, "w_glu", [512, 512], F32, "ExternalInput")
    bglu = _dram(nc, "bglu", [128, 4], F32, "ExternalInput")
    wab = _dram(nc, "wab", [512, D], F32, "ExternalInput")
    wsb = _dram(nc, "wsb", [512, D], F32, "ExternalInput")
    wo = _dram(nc, "wo", [D, D], F32, "ExternalInput")
    g2bc = _dram(nc, "g2bc", [128, D], F32, "ExternalInput")
    gfbc = _dram(nc, "gfbc", [128, D], F32, "ExternalInput")
    wg = _dram(nc, "wg", [NE, D, FF], F32, "ExternalInput")
    wu = _dram(nc, "wu", [NE, D, FF], F32, "ExternalInput")
    wd = _dram(nc, "wd", [NE, FF, D], F32, "ExternalInput")
    if moe:
        wr = _dram(nc, "wr", [D, 8], F32, "ExternalInput")
    yT = _dram(nc, "yT_scr", [512, TOK], F32, "Internal")
    out = _dram(nc, "out", [TOK, D], F32, "ExternalOutput")

    P = _prog(nc)
    wglu_s = P.sb("wglu_s", [128, 4, 512], BF16)
    wab_s = P.sb("wab_s", [128, 4, D], BF16)
    wsb_s = P.sb("wsb_s", [128, 4, D], BF16)
    wo_s = P.sb("wo_s", [128, 8, D], BF16)
    bglu_s = P.sb("bglu_s", [128, 4], F32)
    g2_s = P.sb("g2_s", [128, D], F32)
    gf_s = P.sb("gf_s", [128, D], F32)
    idf = P.sb("idf", [128, 128], F32)
    idb = P.sb("idb", [128, 128], BF16)
    sPt = P.sb("sPt", [128, 3, 16], F32)
    tP = P.sb("tP", [128, 8, 16], F32)
    LK = P.sb("LK", [128, 3, 13, 16], F32)
    sCt = P.sb("sCt", [128, 2, 16, 16], F32)
    Cblk = P.sb("Cblk", [128, 16 if carry else 1, 2, 128], BF16)
    Fp = P.sb("Fp", [128, 3, 16, 2], F32)
    Xin = P.sb("Xin", [128, 4, 16], F32)
    wdt_full = P.sb("wdt", [128, 32, 512], BF16)
    wdt = wdt_full[:, 0:NF, :]
    wgu4 = [[P.sb("wgu%d%d" % (i, j), [128, 8, 256], BF16)[:] for j in range(2)] for i in range(2)]
    actT = P.sb("actT", [128, NF, 512], BF16)
    xm = P.sb("xm", [128, 4, D], F32)
    h2T = P.sb("h2T", [128, 8, 512], BF16)
    h2f = P.sb("h2f", [128, 8, 128], F32)
    ytile = P.sb("ytile", [128, 4, 512], F32)
    ytmp = P.sb("ytmp", [128, 4, 512], F32)
    junk = ytmp[:, 0:2, :].rearrange("p a t -> p (a t)")
    xs = ytmp[:, 2:4, :].rearrange("p a t -> p (a t)")
    zb = P.sb("zb", [128, 4, 512], BF16)
    zz = P.sb("zz", [128, 4, 512], BF16)
    oa2 = [P.sb("oa%d" % i, [128, 3, 520], F32) for i in range(2 if not carry else 1)]
    onb = P.sb("onb", [128, 8, 64], BF16)
    aoT = P.sb("aoT", [128, 4, 512], BF16)
    sgt2 = [P.sb("sgt%d" % i, [128, 2, 512], F32) for i in range(2 if not carry else 1)]
    mtmp = P.sb("mtmp", [128, 512], F32)
    mT = ytile[:].rearrange("p a t -> p (a t)").bitcast(BF16).rearrange("p (k t) -> p k t", k=8)
    small = P.sb("small", [128, 16], F32)
    gat = P.sb("gat", [128, 4, 8], F32)
    lg = P.sb("lg", [128, 4, 8], F32)
    if moe:
        wr_s = P.sb("wr_s", [128, 8, 8], F32)
    Gf = wdt_full[:].rearrange("p f c -> p (f c)").bitcast(F32)[:, 0:8192].rearrange("p (h t) -> p h t", h=2)
    Gb = actT[:, 0:16, :].rearrange("p a t -> p (a t)").rearrange("p (h t) -> p h t", h=2)
    ych = xm[:].rearrange("p a d -> p (a d)")
    pb = [P.ps("pb%d" % i, [128, 512]) for i in range(4)]
    pacc = [P.ps("pacc%d" % i, [128, 512]) for i in range(2)]
    ptp = P.ps("ptp", [128, 8, 128])
    pacc4 = [pacc[0][:], pacc[1][:], ptp[:, 0:4, :].rearrange("p k t -> p (k t)"), ptp[:, 4:8, :].rearrange("p k t -> p (k t)")]
    qb = [0, (NF + 3) // 4, (NF + 3) // 4 + (NF + 2) // 4, (NF + 3) // 4 + (NF + 2) // 4 + (NF + 1) // 4, NF]
    Bw = Buf(); Bpar = Buf(); BC = Buf(); BG = Buf(); BGb = Buf(); Bych = Buf(); ByT = Buf()
    Bpb = [Buf() for _ in range(4)]; Bpacc = [Buf(), Buf()]; Bptp = Buf()
    Bpacc4 = [Bpacc[0], Bpacc[1], Bptp, Bptp]
    Bwdt = Buf(); Bwgu4 = [[Buf(), Buf()], [Buf(), Buf()]]; Bwdq = [Buf() for _ in range(4)]; Bact = Buf(); Bxm = Buf(); Bh2T = Buf(); Bh2f = Buf()
    Byt = Buf(); Bytmp = Buf(); Bzb = Buf(); Bzz = Buf(); Boa2 = [Buf(), Buf()]; Bon = Buf(); BaoT = Buf(); Bsg2 = [Buf(), Buf()]
    Bmt = Buf(); BmT = Byt; Bsm = Buf(); Bgat = Buf(); Bjunk = Bytmp; Bxs = Bytmp
    outs = []
    cnt = {"pb": 0, "gu": 0}

    def nxt():
        i = cnt["pb"] % 4
        cnt["pb"] += 1
        return i

    P.dma("pool", wglu_s[:], w_glu.rearrange("(k p) c -> p k c", p=128), writes=[Bw])
    P.dma("pool", wab_s[:], wab.rearrange("(k p) c -> p k c", p=128), writes=[Bw])
    P.dma("pool", wsb_s[:], wsb.rearrange("(k p) c -> p k c", p=128), writes=[Bw])
    P.dma("pool", wo_s[:], wo.rearrange("(k p) c -> p k c", p=128), writes=[Bw])
    P.dma("pool", idb[:], ident, writes=[Bw])
    for dst, src in ((bglu_s[:], bglu), (g2_s[:], g2bc), (gf_s[:], gfbc), (idf[:], ident), (sPt[:], ssmP),
                     (sCt[:], ssmC)):
        P.dma("act", dst, src, writes=[Bpar])
    if not carry:
        pass
    elif fprev is None:
        P.dma("act", Fp[:], Fprev, writes=[Bpar])
    else:
        P.dve(lambda e: e.memset(Fp[:], 0.0), writes=[Bpar])
        for d_, fa in enumerate(fprev):
            if fa is not None:
                P.dma("act", Fp[:, d_], fa, writes=[Bpar])
    if moe:
        P.dma("act", wr_s[:], wr.rearrange("(k p) e -> p k e", p=128), writes=[Bpar])
    rb, wbp = [Bpar], [Bpar]
    dv = (lambda f: P.dve(f, reads=rb, writes=wbp)) if carry else (lambda f: None)
    TP = lambda i: tP[:, i]
    if carry:
      emit_lb(P, sPt[:, 0], sPt[:, 1], sPt[:, 2], [TP(0), TP(1), TP(2), TP(3)], LK[:, 0, 0], LK[:, 1, 0], rb, wbp)
    for k in range(12):
        dv(lambda e, k=k: e.tensor_tensor(out=TP(0), in0=LK[:, 0, k], in1=LK[:, 0, k], op=ALU.mult))
        dv(lambda e, k=k: e.tensor_tensor(out=TP(1), in0=LK[:, 1, k], in1=LK[:, 1, k], op=ALU.mult))
        dv(lambda e, k=k: e.tensor_tensor(out=LK[:, 0, k + 1], in0=TP(0), in1=TP(1), op=ALU.subtract))
        dv(lambda e, k=k: e.scalar_tensor_tensor(out=LK[:, 1, k + 1], in0=LK[:, 0, k], scalar=2.0, in1=LK[:, 1, k],
                                                  op0=ALU.mult, op1=ALU.mult))
    dv(lambda e: e.tensor_scalar(out=LK[:, 2], in0=LK[:, 1], scalar1=-1.0, scalar2=None, op0=ALU.mult))

    def cmul(o_r, o_i, a_r, a_i, b_r, b_i):
        dv(lambda e: e.tensor_tensor(out=TP(4), in0=a_r, in1=b_r, op=ALU.mult))
        dv(lambda e: e.tensor_tensor(out=TP(5), in0=a_i, in1=b_i, op=ALU.mult))
        dv(lambda e: e.tensor_tensor(out=TP(6), in0=a_r, in1=b_i, op=ALU.mult))
        dv(lambda e: e.tensor_tensor(out=TP(7), in0=a_i, in1=b_r, op=ALU.mult))
        dv(lambda e: e.tensor_tensor(out=o_r, in0=TP(4), in1=TP(5), op=ALU.subtract))
        dv(lambda e: e.tensor_tensor(out=o_i, in0=TP(6), in1=TP(7), op=ALU.add))
    Xr, Xi, G0r, G0i = Xin[:, 0], Xin[:, 1], Xin[:, 2], Xin[:, 3]
    L4r, L4i = LK[:, 0, 12], LK[:, 1, 12]
    cmul(Xr, Xi, Fp[:, 2, :, 0], Fp[:, 2, :, 1], L4r, L4i)
    dv(lambda e: e.tensor_tensor(out=Xr, in0=Xr, in1=Fp[:, 1, :, 0], op=ALU.add))
    dv(lambda e: e.tensor_tensor(out=Xi, in0=Xi, in1=Fp[:, 1, :, 1], op=ALU.add))
    cmul(G0r, G0i, Xr, Xi, L4r, L4i)
    dv(lambda e: e.tensor_tensor(out=Xr, in0=G0r, in1=Fp[:, 0, :, 0], op=ALU.add))
    dv(lambda e: e.tensor_tensor(out=Xi, in0=G0i, in1=Fp[:, 0, :, 1], op=ALU.add))
    cmul(G0r, G0i, Xr, Xi, LK[:, 0, 0], LK[:, 1, 0])
    if carry:
        P.dve(lambda e: e.memset(Cblk[:], 0.0), writes=[BC])
    for Pp in range(16 if carry else 0):
        q = Pp % 4
        for g2 in range(2):
            c0 = 32 * q + 16 * g2
            ps_ = slice(64 * g2, 64 * g2 + 64)
            P.dve(lambda e, Pp=Pp, c0=c0, ps_=ps_: e.tensor_copy(out=Cblk[ps_, Pp, 0, c0:c0 + 16], in_=sCt[ps_, 0, Pp, :]),
                  reads=rb, writes=[BC])
            P.dve(lambda e, Pp=Pp, c0=c0, ps_=ps_: e.tensor_scalar(
                out=Cblk[ps_, Pp, 1, c0:c0 + 16], in0=sCt[ps_, 1, Pp, :], scalar1=-1.0, scalar2=None, op0=ALU.mult),
                reads=rb, writes=[BC])
    for Pp in range(16 if (stage >= 1 and carry) else 0):
        gq, q = Pp // 4, Pp % 4
        gw = lambda f: P.dve(f, reads=[Bpar], writes=[BG] + Bwdq)
        gw(lambda e, Pp=Pp: e.tensor_copy(out=Gf[:, 0, 0:1], in_=G0r[:, Pp:Pp + 1]))
        gw(lambda e, Pp=Pp: e.tensor_copy(out=Gf[:, 1, 0:1], in_=G0i[:, Pp:Pp + 1]))
        for s in range(12):
            k = 1 << s
            lr, li, nli = LK[:, 0, s, Pp:Pp + 1], LK[:, 1, s, Pp:Pp + 1], LK[:, 2, s, Pp:Pp + 1]
            gw(lambda e, k=k, lr=lr: e.tensor_scalar(out=Gf[:, 0, k:2 * k], in0=Gf[:, 0, 0:k], scalar1=lr, scalar2=None,
                                                     op0=ALU.mult))
            gw(lambda e, k=k, nli=nli: e.scalar_tensor_tensor(out=Gf[:, 0, k:2 * k], in0=Gf[:, 1, 0:k], scalar=nli,
                                                              in1=Gf[:, 0, k:2 * k], op0=ALU.mult, op1=ALU.add))
            gw(lambda e, k=k, li=li: e.tensor_scalar(out=Gf[:, 1, k:2 * k], in0=Gf[:, 0, 0:k], scalar1=li, scalar2=None,
                                                     op0=ALU.mult))
            gw(lambda e, k=k, lr=lr: e.scalar_tensor_tensor(out=Gf[:, 1, k:2 * k], in0=Gf[:, 1, 0:k], scalar=lr,
                                                            in1=Gf[:, 1, k:2 * k], op0=ALU.mult, op1=ALU.add))
        P.act(lambda e: e.copy(out=Gb, in_=Gf), reads=[BG] + Bwdq, writes=[BGb, Bact])
        if q == 0:
            P.dma("sp", ych, ylocT[gq * 128:(gq + 1) * 128, :], writes=[Bych, Bxm])
        for tg in range(8):
            i = nxt()
            cs = slice(tg * 512, (tg + 1) * 512)

            def ymm(e, i=i, Pp=Pp, cs=cs):
                e.matmul(pb[i][:], lhsT=Cblk[:, Pp, 0, :], rhs=Gb[:, 0, cs], start=True, stop=False)
                return e.matmul(pb[i][:], lhsT=Cblk[:, Pp, 1, :], rhs=Gb[:, 1, cs], start=False, stop=True)
            P.pe(ymm, reads=[BC, BGb], writes=[Bpb[i]])
            P.dve(lambda e, i=i, cs=cs: e.tensor_tensor(out=ych[:, cs], in0=ych[:, cs], in1=pb[i][:], op=ALU.add),
                  reads=[Bpb[i]], writes=[Bych, Bxm])
        if q == 3:
            P.dma("sp", yT[gq * 128:(gq + 1) * 128, :], ych, reads=[Bych], writes=[ByT])

    for tg in range(8 if stage >= 2 else 0):
        ts_ = slice(tg * 512, (tg + 1) * 512)
        P.dma("sp", ytile[:], (yT if carry else ylocT)[:, ts_].rearrange("(k p) t -> p k t", p=128), reads=[ByT], writes=[Byt])
        P.dve(lambda e: e.tensor_tensor(out=ytmp[:], in0=ytile[:], in1=ytile[:], op=ALU.mult), reads=[Byt], writes=[Bytmp])
        P.dve(lambda e: e.tensor_scalar(out=ytmp[:], in0=ytmp[:], scalar1=0.044715, scalar2=1.0, op0=ALU.mult, op1=ALU.add),
              writes=[Bytmp])
        P.dve(lambda e: e.tensor_tensor(out=ytmp[:], in0=ytmp[:], in1=ytile[:], op=ALU.mult), reads=[Byt], writes=[Bytmp])
        P.act(lambda e: e.activation(out=ytmp[:], in_=ytmp[:], func=AF.Sigmoid, scale=1.5957691216), writes=[Bytmp])
        P.dve(lambda e: e.tensor_tensor(out=zb[:], in0=ytmp[:], in1=ytile[:], op=ALU.mult), reads=[Byt, Bytmp], writes=[Bzb])
        for oc in range(4):
            i = nxt()

            def glu(e, i=i, oc=oc):
                for kc in range(4):
                    ins_ = e.matmul(pb[i][:], lhsT=wglu_s[:, kc, oc * 128:(oc + 1) * 128], rhs=zb[:, kc, :],
                                    start=(kc == 0), stop=(kc == 3))
                return ins_
            P.pe(glu, reads=[Bw, Bzb], writes=[Bpb[i]])
            P.act(lambda e, i=i, oc=oc: e.activation(out=ytmp[:, oc, :], in_=pb[i][:], func=AF.Sigmoid,
                                                     bias=bglu_s[:, oc:oc + 1]), reads=[Bpb[i], Bpar], writes=[Bytmp])
        P.dve(lambda e: e.tensor_tensor(out=zz[:], in0=zb[:], in1=ytmp[:], op=ALU.mult), reads=[Bzb, Bytmp], writes=[Bzz])
        if stage < 3:
            continue
        for tl in range(4):
            t0 = tg * 512 + tl * 128
            oa = oa2[tl % len(oa2)]
            Boa = Boa2[tl % len(oa2)]
            P.dma("act", oa[:], oacc[:, t0:t0 + 128, :].rearrange("g p f -> p g f"), writes=[Boa])
            P.dve(lambda e, oa=oa: e.tensor_tensor(out=oa[:, 0, :], in0=oa[:, 0, :], in1=oa[:, 1, :], op=ALU.add), writes=[Boa])
            P.dve(lambda e, oa=oa: e.tensor_tensor(out=oa[:, 0, :], in0=oa[:, 0, :], in1=oa[:, 2, :], op=ALU.add), writes=[Boa])
            U = oa[:, 0, :].rearrange("p (j d) -> p j d", d=65)
            P.dve(lambda e, U=U: e.reciprocal(out=small[:, 0:8], in_=U[:, :, 64]), reads=[Boa], writes=[Bsm])
            P.dve(lambda e, U=U: e.tensor_tensor(out=onb[:], in0=U[:, :, 0:64],
                                                 in1=small[:, 0:8].unsqueeze(2).to_broadcast([128, 8, 64]), op=ALU.mult),
                  reads=[Boa, Bsm], writes=[Bon])
            onf = onb[:].rearrange("p j d -> p (j d)")
            ptb_ = ptp[:].rearrange("p k t -> p (k t)").bitcast(BF16)[:, 0:512].rearrange("p (k t) -> p k t", k=4)

            def otr(e, onf=onf, ptb_=ptb_):
                for kc in range(4):
                    ins_ = e.transpose(out=ptb_[:, kc, :], in_=onf[:, kc * 128:(kc + 1) * 128], identity=idb[:])
                return ins_
            P.pe(otr, reads=[Bon, Bw], writes=[Bptp])
            P.act(lambda e, tl=tl, ptb_=ptb_: e.copy(out=aoT[:, :, tl * 128:(tl + 1) * 128], in_=ptb_),
                  reads=[Bptp], writes=[BaoT])
        if stage < 4:
            continue
        for oc in range(8):
            sgt = sgt2[oc % len(sgt2)]
            Bsg = Bsg2[oc % len(sgt2)]
            P.dma("act", sgt[:], sgT[:, ts_].rearrange("(a k p) t -> p a k t", a=2, p=128)[:, :, oc, :], writes=[Bsg])
            ia, ib = nxt(), nxt()

            def br(e, ia=ia, ib=ib, oc=oc):
                for kc in range(4):
                    e.matmul(pb[ia][:], lhsT=wab_s[:, kc, oc * 128:(oc + 1) * 128], rhs=aoT[:, kc, :],
                             start=(kc == 0), stop=(kc == 3))
                for kc in range(4):
                    ins_ = e.matmul(pb[ib][:], lhsT=wsb_s[:, kc, oc * 128:(oc + 1) * 128], rhs=zz[:, kc, :],
                                    start=(kc == 0), stop=(kc == 3))
                return ins_
            P.pe(br, reads=[Bw, BaoT, Bzz], writes=[Bpb[ia], Bpb[ib]])
            P.dve(lambda e, ia=ia, sgt=sgt: e.tensor_tensor(out=mtmp[:], in0=pb[ia][:], in1=sgt[:, 0, :], op=ALU.mult),
                  reads=[Bpb[ia], Bsg], writes=[Bmt])
            P.dve(lambda e, ib=ib, sgt=sgt: e.tensor_tensor(out=sgt[:, 1, :], in0=pb[ib][:], in1=sgt[:, 1, :], op=ALU.mult),
                  reads=[Bpb[ib]], writes=[Bsg])
            P.dve(lambda e, oc=oc, sgt=sgt: e.tensor_tensor(out=mT[:, oc, :], in0=mtmp[:], in1=sgt[:, 1, :], op=ALU.add),
                  reads=[Bmt, Bsg], writes=[BmT])
        if stage < 5:
            continue
        for tl in range(4):
            t0 = tg * 512 + tl * 128
            P.dma("sp", xm[:, tl, :], x[t0:t0 + 128, :], writes=[Bxm])
            for hf in range(2):
                i = nxt()

                def om(e, i=i, tl=tl, hf=hf):
                    for kc in range(8):
                        ins_ = e.matmul(pb[i][:], lhsT=mT[:, kc, tl * 128:(tl + 1) * 128],
                                        rhs=wo_s[:, kc, hf * 512:(hf + 1) * 512], start=(kc == 0), stop=(kc == 7))
                    return ins_
                P.pe(om, reads=[BmT, Bw], writes=[Bpb[i]])
                P.dve(lambda e, i=i, tl=tl, hf=hf: e.tensor_tensor(out=xm[:, tl, hf * 512:(hf + 1) * 512],
                                                                   in0=xm[:, tl, hf * 512:(hf + 1) * 512], in1=pb[i][:],
                                                                   op=ALU.add), reads=[Bpb[i]], writes=[Bxm])
            P.act(lambda e, tl=tl: e.activation(out=junk, in_=xm[:, tl, :], func=AF.Square), reads=[Bxm], writes=[Bjunk])
            P.dve(lambda e: e.reduce_sum(out=small[:, 8:9], in_=junk, axis=AX.X), reads=[Bjunk], writes=[Bsm])
            P.act(lambda e: e.activation(out=small[:, 9:10], in_=small[:, 8:9], func=AF.Sqrt, bias=EPS, scale=1.0 / D),
                  writes=[Bsm])
            P.dve(lambda e: e.reciprocal(out=small[:, 9:10], in_=small[:, 9:10]), writes=[Bsm])
            P.dve(lambda e, tl=tl: e.scalar_tensor_tensor(out=xs, in0=xm[:, tl, :], scalar=small[:, 9:10], in1=g2_s[:],
                                                          op0=ALU.mult, op1=ALU.mult), reads=[Bxm, Bsm, Bpar], writes=[Bxs])

            def tr(e):
                for kc in range(8):
                    ins_ = e.transpose(out=ptp[:, kc, :], in_=xs[:, kc * 128:(kc + 1) * 128], identity=idf[:])
                return ins_
            P.pe(tr, reads=[Bxs, Bpar], writes=[Bptp])
            P.act(lambda e, tl=tl: e.copy(out=h2T[:, :, tl * 128:(tl + 1) * 128], in_=ptp[:]), reads=[Bptp], writes=[Bh2T])
            if moe:
                P.dve(lambda e: e.tensor_copy(out=h2f[:], in_=ptp[:]), reads=[Bptp, Bh2T], writes=[Bh2f])
                if stage < 5.2:
                    continue
                i = nxt()

                def rt(e, i=i):
                    for kc in range(8):
                        ins_ = e.matmul(pb[i][:, 0:8], lhsT=h2f[:, kc, :], rhs=wr_s[:, kc, :], start=(kc == 0), stop=(kc == 7))
                    return ins_
                P.pe(rt, reads=[Bh2f, Bpar], writes=[Bpb[i]])
                if stage < 5.3:
                    continue
                L_ = lg[:, tl, :]
                G_ = gat[:, tl, :]
                s_ = lambda a: small[:, a:a + 1]
                gd = lambda f, rd=(): P.dve(f, reads=list(rd), writes=[Bgat])
                gd(lambda e, i=i, L_=L_: e.tensor_copy(out=L_, in_=pb[i][:, 0:8]), [Bpb[i]])
                gd(lambda e, L_=L_: e.reduce_max(out=s_(10), in_=L_, axis=AX.X))
                gd(lambda e, L_=L_, G_=G_: e.tensor_scalar(out=G_, in0=L_, scalar1=s_(10), scalar2=None, op0=ALU.is_equal))
                gd(lambda e, L_=L_, G_=G_: e.scalar_tensor_tensor(out=L_, in0=G_, scalar=-1e30, in1=L_, op0=ALU.mult, op1=ALU.add))
                if stage < 5.4:
                    continue
                gd(lambda e, L_=L_: e.reduce_max(out=s_(11), in_=L_, axis=AX.X))
                gd(lambda e, L_=L_: e.tensor_scalar(out=L_, in0=L_, scalar1=s_(11), scalar2=None, op0=ALU.is_equal))
                gd(lambda e: e.tensor_tensor(out=s_(12), in0=s_(11), in1=s_(10), op=ALU.subtract))
                if stage < 5.5:
                    continue
                P.act(lambda e: e.activation(out=s_(12), in_=s_(12), func=AF.Exp), writes=[Bgat])
                gd(lambda e: e.tensor_scalar(out=s_(13), in0=s_(12), scalar1=1.0, scalar2=None, op0=ALU.add))
                gd(lambda e: e.reciprocal(out=s_(13), in_=s_(13)))
                gd(lambda e: e.tensor_tensor(out=s_(14), in0=s_(12), in1=s_(13), op=ALU.mult))
                gd(lambda e, G_=G_: e.tensor_scalar(out=G_, in0=G_, scalar1=s_(13), scalar2=None, op0=ALU.mult))
                gd(lambda e, L_=L_, G_=G_: e.scalar_tensor_tensor(out=G_, in0=L_, scalar=s_(14), in1=G_, op0=ALU.mult, op1=ALU.add))
        if stage < 6:
            continue
        for ex in range(NE):
            for f0 in range(0, NF, 2):
                wset = cnt["gu"] % 2
                cnt["gu"] += 1
                wgs, wus = wgu4[wset][0], wgu4[wset][1]
                P.dma("pool", wgs, wg[ex, :, f0 * 128:(f0 + 2) * 128].rearrange("(k p) c -> p k c", p=128),
                      writes=[Bwgu4[wset][0]])
                P.dma("pool", wus, wu[ex, :, f0 * 128:(f0 + 2) * 128].rearrange("(k p) c -> p k c", p=128),
                      writes=[Bwgu4[wset][1]])
                for fc in range(2):
                    ig, iu = nxt(), nxt()

                    def gu(e, ig=ig, iu=iu, fc=fc, wgs=wgs, wus=wus):
                        for kc in range(8):
                            e.matmul(pb[ig][:], lhsT=wgs[:, kc, fc * 128:(fc + 1) * 128], rhs=h2T[:, kc, :],
                                     start=(kc == 0), stop=(kc == 7))
                        for kc in range(8):
                            ins_ = e.matmul(pb[iu][:], lhsT=wus[:, kc, fc * 128:(fc + 1) * 128], rhs=h2T[:, kc, :],
                                            start=(kc == 0), stop=(kc == 7))
                        return ins_
                    P.pe(gu, reads=[Bwgu4[wset][0], Bwgu4[wset][1], Bh2T], writes=[Bpb[ig], Bpb[iu]])
                    P.act(lambda e, ig=ig: e.activation(out=mtmp[:], in_=pb[ig][:], func=AF.Silu), reads=[Bpb[ig]], writes=[Bmt])
                    P.dve(lambda e, iu=iu, f=f0 + fc: e.tensor_tensor(out=actT[:, f, :], in0=mtmp[:], in1=pb[iu][:], op=ALU.mult),
                          reads=[Bmt, Bpb[iu]], writes=[Bact])
            for hf in range(2):
                for qi in range(4):
                    fa, fb = qb[qi], qb[qi + 1]
                    wq = wdt_full[:, 8 * qi:8 * qi + (fb - fa), :]
                    P.dma("pool", wq, wd[ex, fa * 128:fb * 128, hf * 512:(hf + 1) * 512].rearrange("(f p) c -> p f c", p=128),
                          writes=[Bwdq[qi]])
                    for tl in range(4):
                        def dn(e, tl=tl, fa=fa, fb=fb, wq=wq):
                            for f in range(fa, fb):
                                ins_ = e.matmul(pacc4[tl], lhsT=actT[:, f, tl * 128:(tl + 1) * 128], rhs=wq[:, f - fa, :],
                                                start=(f == 0), stop=(f == NF - 1))
                            return ins_
                        P.pe(dn, reads=[Bact, Bwdq[qi]], writes=[Bpacc4[tl]])
                for tl in range(4):
                    xsl = xm[:, tl, hf * 512:(hf + 1) * 512]
                    if moe:
                        P.dve(lambda e, xsl=xsl, tl=tl, ex=ex: e.scalar_tensor_tensor(
                            out=xsl, in0=pacc4[tl], scalar=gat[:, tl, ex:ex + 1], in1=xsl, op0=ALU.mult, op1=ALU.add),
                            reads=[Bpacc4[tl], Bgat], writes=[Bxm])
                    else:
                        P.dve(lambda e, xsl=xsl, tl=tl: e.tensor_tensor(out=xsl, in0=xsl, in1=pacc4[tl], op=ALU.add),
                              reads=[Bpacc4[tl]], writes=[Bxm])
        if stage < 7:
            continue
        for tl in range(4):
            t0 = tg * 512 + tl * 128
            if moe:
                P.act(lambda e, tl=tl: e.activation(out=junk, in_=xm[:, tl, :], func=AF.Square), reads=[Bxm], writes=[Bjunk])
                P.dve(lambda e: e.reduce_sum(out=small[:, 8:9], in_=junk, axis=AX.X), reads=[Bjunk], writes=[Bsm])
                P.act(lambda e: e.activation(out=small[:, 9:10], in_=small[:, 8:9], func=AF.Sqrt, bias=EPS, scale=1.0 / D),
                      writes=[Bsm])
                P.dve(lambda e: e.reciprocal(out=small[:, 9:10], in_=small[:, 9:10]), writes=[Bsm])
                P.dve(lambda e, tl=tl: e.scalar_tensor_tensor(out=xs, in0=xm[:, tl, :], scalar=small[:, 9:10], in1=gf_s[:],
                                                              op0=ALU.mult, op1=ALU.mult), reads=[Bxm, Bsm, Bpar], writes=[Bxs])
                outs.append(P.dma("sp", out[t0:t0 + 128, :], xs, reads=[Bxs]))
            else:
                outs.append(P.dma("sp", out[t0:t0 + 128, :], xm[:, tl, :], reads=[Bxm]))
    P.fence("sp", outs)
    _finish(P)
    return nc


def _t5_bucket(dist):
    max_exact = 16
    d = np.maximum(dist, 1).astype(np.float64)
    large = max_exact + (np.log(d / max_exact) / np.log(2048 / max_exact) * (32 - max_exact)).astype(np.int32)
    large = np.minimum(large, 31)
    return np.where(dist < max_exact, dist, large).astype(np.int32)


def prep_B1_tables(inp):
    rel = np.asarray(inp["rel_bias"], np.float32)
    kk = np.arange(128)[:, None]
    qi = np.arange(128)[None, :]
    biasT = np.zeros((128, 24, 2, 128), np.float32)
    maskT = np.zeros((128, 2, 128), np.float32)
    for t in range(2):
        delta = (128 if t == 0 else 0) + qi - kk
        maskT[:, t, :] = ((delta >= 0) & (delta <= 128)).astype(np.float32)
        for g, (w, r) in enumerate(GROUPS):
            bucket = _t5_bucket(np.clip(delta, 0, 128) * r)
            hp = 8 * g + np.array([0, 2, 1, 3, 4, 6, 5, 7])
            biasT[:, 8 * g:8 * g + 8, t, :] = np.transpose(rel[bucket][:, :, hp], (0, 2, 1))
    return dict(biasT=biasT, maskT=maskT)


def prep_B1(inp, resA):
    tb = prep_B1_tables(inp)
    biasT, maskT = tb["biasT"], tb["maskT"]
    maps = []
    for c in range(8):
        ci = c % 4
        m = dict(biasT=biasT, maskT=maskT, hv=np.full((128, 1), 1.0 if ci > 0 else 0.0, np.float32))
        for g, (w, r) in enumerate(GROUPS):
            L = TOK // r
            own_k = np.asarray(resA[c]["kT"][g]).reshape(512, r, L)
            own_v = np.asarray(resA[c]["v"][g]).reshape(r, L, 512)
            if ci > 0:
                hk = np.asarray(resA[c - 1]["kT"][g]).reshape(512, r, L)[:, :, L - 128:]
                hvv = np.asarray(resA[c - 1]["v"][g]).reshape(r, L, 512)[:, L - 128:, :]
            else:
                hk = np.zeros((512, r, 128), own_k.dtype)
                hvv = np.zeros((r, 128, 512), own_v.dtype)
            m["qT%d" % g] = np.ascontiguousarray(resA[c]["qT"][g])
            m["kTh%d" % g] = np.ascontiguousarray(np.concatenate([hk, own_k], axis=2))
            m["vh%d" % g] = np.ascontiguousarray(np.concatenate([hvv, own_v], axis=1))
        maps.append(m)
    return maps


def prep_B2(inp, l, xs, resA, resB1):
    f = lambda a: np.ascontiguousarray(a, np.float32)
    lay = _ssm_layouts(inp, l)
    common = dict(ssmP=lay["ssmP"], ssmC=lay["ssmC"], ident=np.eye(128, dtype=np.float32),
                  w_glu=f(inp["w_glu"][l]), bglu=f(np.asarray(inp["b_glu"][l]).reshape(4, 128).T),
                  wab=f(inp["w_attn_br"][l]), wsb=f(inp["w_ssm_br"][l]), wo=f(inp["w_out"][l]),
                  g2bc=f(np.broadcast_to(np.asarray(inp["norm2_g"][l]), (128, D))),
                  gfbc=f(np.broadcast_to(np.asarray(inp["final_norm_g"]), (128, D))))
    if l % 2 == 0:
        i = l // 2
        common.update(wg=f(inp["ffn_w_gate"][i][None]), wu=f(inp["ffn_w_up"][i][None]), wd=f(inp["ffn_w_down"][i][None]))
    else:
        i = l // 2
        common.update(wg=f(inp["moe_w_gate"][i]), wu=f(inp["moe_w_up"][i]), wd=f(inp["moe_w_down"][i]),
                      wr=f(inp["moe_router"][i]))
    maps = []
    for c in range(8):
        ci = c % 4
        Fprev = np.zeros((128, 3, 16, 2), np.float32)
        for d in range(3):
            if ci - 1 - d >= 0:
                Fprev[:, d] = resA[c - 1 - d]["F"]
        maps.append(dict(common, x=f(xs[c]), oacc=f(resB1[c]["oacc"]), sgT=f(resA[c]["sgT"]),
                         ylocT=f(resA[c]["ylocT"]), Fprev=Fprev))
    return maps


NCH = 4
SEQ = NCH * TOK
ARENA_BYTES = 212800


def build_fused(nc):
    global _OVR, _PROG
    I = lambda name, shape, dt=F32: nc.dram_tensor(name, list(shape), dt, kind="ExternalInput").ap()
    S = lambda name, shape, dt=F32: nc.dram_tensor(name, list(shape), dt, kind="Internal").ap()
    x = I("x", [SEQ, D])
    hvv = I("hvv", [NCH, 128, 1])
    out = nc.dram_tensor("out", [TOK, D], F32, kind="ExternalOutput").ap()
    com = dict(ident=I("ident", [128, 128]), masks=I("masks", [128, 8]), biasT=I("biasT", [128, 24, 2, 128]),
               maskT=I("maskT", [128, 2, 128]), gfbc=I("gfbc", [128, D]))
    lay = []
    for l in range(2):
        lay.append(dict(gbc=I("gbc%d" % l, [128, D]), w_in=I("w_in%d" % l, [D, 7168]),
                        ssmB=I("ssmB%d" % l, [128, 5, 4, 64]), ssmP=I("ssmP%d" % l, [128, 3, 16]),
                        ssmC=I("ssmC%d" % l, [128, 2, 16, 16]), dsk=I("dsk%d" % l, [128, 4]),
                        w_glu=I("w_glu%d" % l, [512, 512]), bglu=I("bglu%d" % l, [128, 4]),
                        wab=I("wab%d" % l, [512, D]), wsb=I("wsb%d" % l, [512, D]), wo=I("wo%d" % l, [D, D]),
                        g2bc=I("g2bc%d" % l, [128, D])))
    ffn = [dict(wg=I("wg0", [1, D, 2816]), wu=I("wu0", [1, D, 2816]), wd=I("wd0", [1, 2816, D])),
           dict(wg=I("wg1", [8, D, 3584]), wu=I("wu1", [8, D, 3584]), wd=I("wd1", [8, 3584, D]), wr=I("wr", [D, 8]))]
    x1 = S("x1_scr", [SEQ, D])
    ck = []
    for k in range(NCH):
        ck.append(dict(qT=S("qT_%d" % k, [3, 512, TOK], BF16), kT=S("kT_%d" % k, [3, 512, TOK], BF16),
                       v=S("v_%d" % k, [3, TOK, 512], BF16), sgT=S("sgT_%d" % k, [2048, TOK]),
                       ylocT=S("ylocT_%d" % k, [512, TOK]), F=S("F_%d" % k, [128, 16, 2]),
                       oacc=S("oacc_%d" % k, [3, TOK, 520]), yT_scr=S("yT_%d" % k, [512, TOK])))
    P = Prog(nc, arena_bytes=ARENA_BYTES)
    _PROG = P
    try:
        for l in range(2):
            xin = x if l == 0 else x1
            L_ = lay[l]
            own = list(range(NCH)) if l == 0 else [NCH - 1]
            for k in range(NCH):
                c = ck[k]
                _OVR = dict(com, x=xin[k * TOK:(k + 1) * TOK, :], gbc=L_["gbc"], w_in=L_["w_in"], ssmB=L_["ssmB"],
                            ssmP=L_["ssmP"], ssmC=L_["ssmC"], dsk=L_["dsk"], qT=c["qT"], kT=c["kT"], v=c["v"],
                            sgT=c["sgT"], ylocT=c["ylocT"], F=c["F"])
                if k > 0:
                    _OVR["Fin"] = ck[k - 1]["F"]
                build_A(nc, mode="full" if (l == 0 or k == NCH - 1) else ("kvu" if k == NCH - 2 else "u"), prev=(k > 0))
            for k in own:
                c = ck[k]
                p = ck[k - 1] if k > 0 else None
                _OVR = dict(com, oacc=c["oacc"])
                build_B1(nc, fz=dict(qT=c["qT"], kT=c["kT"], v=c["v"], kT_prev=p["kT"] if p else None,
                                     v_prev=p["v"] if p else None, hv=hvv[k]))
            for k in own:
                c = ck[k]
                xo = x1[k * TOK:(k + 1) * TOK, :] if l == 0 else out
                _OVR = dict(com, x=xin[k * TOK:(k + 1) * TOK, :], out=xo, oacc=c["oacc"],
                            sgT=c["sgT"], ylocT=c["ylocT"], yT_scr=c["yT_scr"], ssmP=L_["ssmP"], ssmC=L_["ssmC"],
                            w_glu=L_["w_glu"], bglu=L_["bglu"], wab=L_["wab"], wsb=L_["wsb"], wo=L_["wo"],
                            g2bc=L_["g2bc"], **ffn[l])
                build_B2(nc, moe=(l == 1), fprev=[None, None, None], carry=False)
        P.emit()
        P.close()
    finally:
        _OVR = None
        _PROG = None
    return nc


def prep_fused(inp):
    f = lambda a: np.ascontiguousarray(a, np.float32)
    bc = lambda a: f(np.broadcast_to(np.asarray(a, np.float32), (128, D)))
    b1 = prep_B1_tables(inp)
    com = dict(ident=np.eye(128, dtype=np.float32), biasT=b1["biasT"], maskT=b1["maskT"], gfbc=bc(inp["final_norm_g"]),
               wg0=f(inp["ffn_w_gate"][0][None]), wu0=f(inp["ffn_w_up"][0][None]), wd0=f(inp["ffn_w_down"][0][None]),
               wg1=f(inp["moe_w_gate"][0]), wu1=f(inp["moe_w_up"][0]), wd1=f(inp["moe_w_down"][0]),
               wr=f(inp["moe_router"][0]))
    for l in range(2):
        lay = _ssm_layouts(inp, l)
        com["masks"] = lay["masks"]
        com.update({"gbc%d" % l: bc(inp["norm1_g"][l]), "w_in%d" % l: f(inp["w_in"][l]), "ssmB%d" % l: lay["ssmB"],
                    "ssmP%d" % l: lay["ssmP"], "ssmC%d" % l: lay["ssmC"], "dsk%d" % l: lay["dsk"],
                    "w_glu%d" % l: f(inp["w_glu"][l]), "bglu%d" % l: f(np.asarray(inp["b_glu"][l]).reshape(4, 128).T),
                    "wab%d" % l: f(inp["w_attn_br"][l]), "wsb%d" % l: f(inp["w_ssm_br"][l]), "wo%d" % l: f(inp["w_out"][l]),
                    "g2bc%d" % l: bc(inp["norm2_g"][l])})
    x = np.asarray(inp["x"], np.float32)
    maps = []
    for c in range(8):
        b, k = c // NCH, c % NCH
        xp = np.zeros((SEQ, D), np.float32)
        xp[(NCH - 1 - k) * TOK:] = x[b, :(k + 1) * TOK]
        hvv = np.zeros((NCH, 128, 1), np.float32)
        hvv[NCH - k:] = 1.0
        maps.append(dict(com, x=xp, hvv=hvv))
    return maps


def kernel(**inp):
    nc = bass.Bass("TRN2", target_bir_lowering=False)
    build_fused(nc)
    res = run_bass_kernel_spmd(nc, prep_fused(inp), core_ids=list(range(8))).results
    o = np.stack([np.asarray(r["out"], np.float32) for r in res])
    return o.reshape(2, SEQ, D)
```
